# Optimizing a Trainium2 kernel written in Bass

```python
import math
import jax, jax.numpy as jnp
from jax import lax
import numpy as np

D_MODEL = 1024
BATCH = 8
SEQ = 4096
DEPTH = 1

CHUNK = 64
N_MEM = 256
D_MIX = D_MODEL
CONV_CH = D_MIX // 2
SB_HEADS = 8
SB_HEAD_DIM = (D_MIX - CONV_CH) // SB_HEADS
SB_WIDTH = SB_HEADS * SB_HEAD_DIM
CONV_WIDTH = 31
Q_BLOCK = 128
MEM_HEADS = 4
MEM_HEAD_DIM = D_MODEL // MEM_HEADS
N_EXPERTS = 32
TOP_K = 4
D_FF = D_MODEL
SWIGLU_LIMIT = 7.0
SWIGLU_ALPHA = 1.702
EXPERT_BLOCK = 256
LN_EPS = 1e-5
DEEPNORM_ALPHA = (2.0 * DEPTH) ** 0.25
DEEPNORM_BETA = (8.0 * DEPTH) ** -0.25
IN_COLS = 2 * CONV_CH + 3 * SB_WIDTH

kernel_name = 'hybrid_conv_stickbreak_mem_moe_deepnorm'


def layer_norm(x, g, b):
    xf = x.astype(jnp.float32)
    mu = xf.mean(-1, keepdims=True)
    var = jnp.square(xf - mu).mean(-1, keepdims=True)
    y = (xf - mu) * lax.rsqrt(var + LN_EPS) * g.astype(jnp.float32) + b.astype(jnp.float32)
    return y.astype(x.dtype)


def conformer_conv(a, gate, w_dw, b_dw, g, b):
    u = a * jax.nn.sigmoid(gate)
    u = lax.conv_general_dilated(
        u, w_dw[:, None, :], window_strides=(1,), padding=[(CONV_WIDTH - 1, 0)],
        dimension_numbers=('NWC', 'WIO', 'NWC'), feature_group_count=CONV_CH) + b_dw
    return jax.nn.silu(layer_norm(u, g, b))


def stick_breaking_attention(q, k, v):
    T = q.shape[1]
    scale = SB_HEAD_DIM ** -0.5
    outs = []
    for q0 in range(0, T, Q_BLOCK):
        L = q0 + Q_BLOCK
        z = jnp.einsum('bqhd,bkhd->bhqk', q[:, q0:L], k[:, :L]).astype(jnp.float32) * scale
        t_idx = q0 + jnp.arange(Q_BLOCK)[:, None]
        s_idx = jnp.arange(L)[None, :]
        before = s_idx < t_idx
        log_beta = jax.nn.log_sigmoid(z)
        log_rem = jnp.where(before, jax.nn.log_sigmoid(-z), 0.0)
        between = lax.cumsum(log_rem, axis=3, reverse=True) - log_rem
        w = jnp.where(before, jnp.exp(log_beta + between), 0.0)
        outs.append(jnp.einsum('bhqk,bkhd->bqhd', w.astype(v.dtype), v[:, :L]))
    return jnp.concatenate(outs, axis=1)


def memory_cross_attention(h, mem_n, wq, wk, wv, wo):
    B, T, _ = h.shape
    M = mem_n.shape[1]
    q = (h @ wq).reshape(B, T, MEM_HEADS, MEM_HEAD_DIM)
    k = (mem_n @ wk).reshape(B, M, MEM_HEADS, MEM_HEAD_DIM)
    v = (mem_n @ wv).reshape(B, M, MEM_HEADS, MEM_HEAD_DIM)
    s = jnp.einsum('bqhd,bkhd->bhqk', q, k).astype(jnp.float32) * (MEM_HEAD_DIM ** -0.5)
    p = jax.nn.softmax(s, axis=-1).astype(v.dtype)
    o = jnp.einsum('bhqk,bkhd->bqhd', p, v).reshape(B, T, D_MODEL)
    return o @ wo


def routed_experts(h, w_router, b_router, w_gu, b_gu, w_down, b_down):
    B, T, D = h.shape
    xf = h.reshape(-1, D)
    n_tok = xf.shape[0]
    logits = (xf @ w_router + b_router).astype(jnp.float32)
    top_val, top_idx = lax.top_k(logits, TOP_K)
    gates = jax.nn.softmax(top_val, axis=-1)
    n_assign = n_tok * TOP_K
    e_flat = top_idx.reshape(-1).astype(jnp.int32)
    tok_flat = jnp.arange(n_assign, dtype=jnp.int32) // TOP_K
    g_flat = gates.reshape(-1)
    order = jnp.argsort(e_flat)
    e_sorted = e_flat[order]
    counts = jnp.bincount(e_flat, length=N_EXPERTS).astype(jnp.int32)
    padded = (counts + EXPERT_BLOCK - 1) // EXPERT_BLOCK * EXPERT_BLOCK
    start = jnp.cumsum(counts) - counts
    padded_end = jnp.cumsum(padded)
    padded_start = padded_end - padded
    rank = jnp.arange(n_assign, dtype=jnp.int32) - start[e_sorted]
    dest = padded_start[e_sorted] + rank
    n_slots = -(-n_assign // EXPERT_BLOCK) * EXPERT_BLOCK + N_EXPERTS * EXPERT_BLOCK
    n_blocks = n_slots // EXPERT_BLOCK
    slot_tok = jnp.zeros((n_slots,), jnp.int32).at[dest].set(tok_flat[order])
    slot_gate = jnp.zeros((n_slots,), jnp.float32).at[dest].set(g_flat[order])
    block_start = jnp.arange(n_blocks, dtype=jnp.int32) * EXPERT_BLOCK
    block_expert = jnp.minimum(jnp.searchsorted(padded_end, block_start, side='right'),
                               N_EXPERTS - 1).astype(jnp.int32)

    def expert_block(args):
        tok, gate, e = args
        xb = xf[tok]
        gu = xb @ w_gu[e] + b_gu[e]
        g_ = jnp.minimum(gu[:, :D_FF], SWIGLU_LIMIT)
        u_ = jnp.clip(gu[:, D_FF:], -SWIGLU_LIMIT, SWIGLU_LIMIT)
        act = (u_ + 1.0) * g_ * jax.nn.sigmoid(SWIGLU_ALPHA * g_)
        y = act @ w_down[e] + b_down[e]
        return y * gate[:, None].astype(y.dtype)

    ys = lax.map(expert_block, (slot_tok.reshape(n_blocks, EXPERT_BLOCK),
                                slot_gate.reshape(n_blocks, EXPERT_BLOCK), block_expert))
    out = jnp.zeros((n_tok, D), jnp.float32).at[slot_tok].add(
        ys.reshape(n_slots, D).astype(jnp.float32))
    return out.astype(h.dtype).reshape(B, T, D)


def setup_inputs(seed: int = 0) -> dict:
    key = jax.random.key(seed)
    ks = jax.random.split(key, 32)
    f32 = jnp.float32

    def nrm(k, shape, scale):
        return jax.random.normal(k, shape, f32) * scale

    def gain(k, shape):
        return 1.0 + 0.02 * jax.random.normal(k, shape, f32)

    def bias(k, shape):
        return 0.02 * jax.random.normal(k, shape, f32)

    Lr = DEPTH
    return {
        'x': jax.random.normal(ks[0], (BATCH, SEQ, D_MODEL), f32),
        'mem': jax.random.normal(ks[1], (BATCH, N_MEM, D_MODEL), f32),
        'ln_in_g': gain(ks[2], (D_MODEL,)),
        'ln_in_b': bias(ks[3], (D_MODEL,)),
        'ln_mem_g': gain(ks[4], (D_MODEL,)),
        'ln_mem_b': bias(ks[5], (D_MODEL,)),
        'w_in': nrm(ks[6], (Lr, D_MODEL, IN_COLS), D_MODEL ** -0.5),
        'b_in': bias(ks[7], (Lr, IN_COLS)),
        'w_dw': nrm(ks[8], (Lr, CONV_WIDTH, CONV_CH), CONV_WIDTH ** -0.5),
        'b_dw': bias(ks[9], (Lr, CONV_CH)),
        'ln_conv_g': gain(ks[10], (Lr, CONV_CH)),
        'ln_conv_b': bias(ks[11], (Lr, CONV_CH)),
        'w_out': nrm(ks[12], (Lr, D_MIX, D_MODEL), D_MIX ** -0.5 * DEEPNORM_BETA),
        'b_out': bias(ks[13], (Lr, D_MODEL)),
        'ln1_g': gain(ks[14], (Lr, D_MODEL)),
        'ln1_b': bias(ks[15], (Lr, D_MODEL)),
        'w_q_mem': nrm(ks[16], (Lr, D_MODEL, D_MODEL), D_MODEL ** -0.5),
        'w_k_mem': nrm(ks[17], (Lr, D_MODEL, D_MODEL), D_MODEL ** -0.5),
        'w_v_mem': nrm(ks[18], (Lr, D_MODEL, D_MODEL), D_MODEL ** -0.5),
        'w_o_mem': nrm(ks[19], (Lr, D_MODEL, D_MODEL), D_MODEL ** -0.5 * DEEPNORM_BETA),
        'ln2_g': gain(ks[20], (Lr, D_MODEL)),
        'ln2_b': bias(ks[21], (Lr, D_MODEL)),
        'w_router': nrm(ks[22], (Lr, D_MODEL, N_EXPERTS), D_MODEL ** -0.5),
        'b_router': nrm(ks[23], (Lr, N_EXPERTS), 0.01),
        'w_gu': nrm(ks[24], (Lr, N_EXPERTS, D_MODEL, 2 * D_FF), D_MODEL ** -0.5),
        'b_gu': bias(ks[25], (Lr, N_EXPERTS, 2 * D_FF)),
        'w_down': nrm(ks[26], (Lr, N_EXPERTS, D_FF, D_MODEL), D_FF ** -0.5 * DEEPNORM_BETA),
        'b_down': bias(ks[27], (Lr, N_EXPERTS, D_MODEL)),
        'ln3_g': gain(ks[28], (Lr, D_MODEL)),
        'ln3_b': bias(ks[29], (Lr, D_MODEL)),
    }


def reference(x, mem, ln_in_g, ln_in_b, ln_mem_g, ln_mem_b, w_in, b_in, w_dw, b_dw,
              ln_conv_g, ln_conv_b, w_out, b_out, ln1_g, ln1_b, w_q_mem, w_k_mem, w_v_mem,
              w_o_mem, ln2_g, ln2_b, w_router, b_router, w_gu, b_gu, w_down, b_down,
              ln3_g, ln3_b):
    B, T, _ = x.shape
    h = layer_norm(x, ln_in_g, ln_in_b)
    mem_n = layer_norm(mem, ln_mem_g, ln_mem_b)
    splits = [CONV_CH, 2 * CONV_CH, 2 * CONV_CH + SB_WIDTH, 2 * CONV_CH + 2 * SB_WIDTH]
    for l in range(DEPTH):
        proj = h @ w_in[l] + b_in[l]
        c_val, c_gate, q, k, v = jnp.split(proj, splits, axis=-1)
        conv_out = conformer_conv(c_val, c_gate, w_dw[l], b_dw[l], ln_conv_g[l], ln_conv_b[l])
        sb_out = stick_breaking_attention(
            q.reshape(B, T, SB_HEADS, SB_HEAD_DIM),
            k.reshape(B, T, SB_HEADS, SB_HEAD_DIM),
            v.reshape(B, T, SB_HEADS, SB_HEAD_DIM)).reshape(B, T, SB_WIDTH)
        mix = jnp.concatenate([conv_out, sb_out], axis=-1) @ w_out[l] + b_out[l]
        h = layer_norm(DEEPNORM_ALPHA * h + mix, ln1_g[l], ln1_b[l])
        ca = memory_cross_attention(h, mem_n, w_q_mem[l], w_k_mem[l], w_v_mem[l], w_o_mem[l])
        h = layer_norm(DEEPNORM_ALPHA * h + ca, ln2_g[l], ln2_b[l])
        moe = routed_experts(h, w_router[l], b_router[l], w_gu[l], b_gu[l], w_down[l], b_down[l])
        h = layer_norm(DEEPNORM_ALPHA * h + moe, ln3_g[l], ln3_b[l])
    return h
```

```python
import numpy as np
from contextlib import ExitStack
import concourse.bass as bass
import concourse.mybir as mybir
from concourse.bass_utils import run_bass_kernel_spmd
from concourse.bass import IndirectOffsetOnAxis

F32 = mybir.dt.float32
BF16 = mybir.dt.bfloat16
I32 = mybir.dt.int32
AF = mybir.ActivationFunctionType
ALU = mybir.AluOpType
AX = mybir.AxisListType
ENGS = ["tensor", "vector", "scalar", "gpsimd", "sync"]

T = 4096
D = 1024
NM = 256
NE = 32
CAP = 1024
NSLOT = NE * CAP
ALPHA = 2.0 ** 0.25
EPS = 1e-5
NT = T // 128
NST = T // 512
PADL = 30
HALF = CAP // 2


class Buf:
    def __init__(self, name):
        self.name = name
        self.w = None
        self.r = {}
        self.sem = None


class Prog:
    def __init__(self, nc, es):
        self.nc = nc
        self.es = es
        self.q = {e: [] for e in ENGS}
        self.cnt = {}
        self.known = {e: {} for e in ENGS}
        self.sems = {}
        self.nbuf = 0
        for e in ENGS:
            self.newsem("E_" + e)

    def newsem(self, name):
        self.sems[name] = self.es.enter_context(self.nc.semaphore(name))
        self.cnt[name] = 0
        return name

    def buf(self, name=None):
        self.nbuf += 1
        return Buf(name or ("b%d" % self.nbuf))

    def _waits(self, eng, deps):
        for d in deps:
            if d is None:
                continue
            s, v = d
            if self.known[eng].get(s, 0) >= v:
                continue
            self.known[eng][s] = v
            self.q[eng].append(("wait", s, v))

    def _deps(self, eng, reads, writes):
        own = "E_" + eng
        deps = []
        for b in reads:
            if b.w is not None:
                deps.append(b.w)
        for b in writes:
            if b.w is not None and b.w[0] != own:
                deps.append(b.w)
            for s, v in b.r.items():
                if s != own:
                    deps.append((s, v))
        return deps

    def _mark(self, tok, reads, writes):
        for b in reads:
            b.r[tok[0]] = max(b.r.get(tok[0], 0), tok[1])
        for b in writes:
            b.w = tok
            b.r = {}

    def op(self, eng, fn, reads=(), writes=()):
        self._waits(eng, self._deps(eng, reads, writes))
        s = "E_" + eng
        self.cnt[s] += 1
        self.q[eng].append(("op", fn, s, 1))
        tok = (s, self.cnt[s])
        self._mark(tok, reads, writes)
        return tok

    def group(self, eng, fns, reads=(), writes=()):
        self._waits(eng, self._deps(eng, reads, writes))
        s = "E_" + eng
        for fn in fns:
            self.cnt[s] += 1
            self.q[eng].append(("op", fn, s, 1))
        tok = (s, self.cnt[s])
        self._mark(tok, reads, writes)
        return tok

    def dma(self, eng, fn, dst, reads=(), stream=False, war=False):
        if dst.sem is None:
            dst.sem = self.newsem("D_" + dst.name)
        deps = self._deps(eng, reads, () if stream else (dst,))
        if stream and war:
            deps = deps + [(s_, v_) for s_, v_ in dst.r.items()]
        self._waits(eng, deps)
        self.cnt[dst.sem] += 16
        self.q[eng].append(("op", fn, dst.sem, 16))
        tok = (dst.sem, self.cnt[dst.sem])
        for b in reads:
            b.r[tok[0]] = max(b.r.get(tok[0], 0), tok[1])
        if stream:
            dst.w = tok
        else:
            dst.w = tok
            dst.r = {}
        return tok

    def barrier(self):
        for e in ENGS:
            self._waits(e, [(s, v) for s, v in self.cnt.items() if v > 0])

    def flush(self):
        nc = self.nc
        q = self.q
        self.q = {e: [] for e in ENGS}
        sems = self.sems
        with nc.Block() as block:
            def replay(e, name):
                for it in q[name]:
                    if it[0] == "wait":
                        e.wait_ge(sems[it[1]], it[2])
                    else:
                        it[1](e).then_inc(sems[it[2]], it[3])

            @block.sync
            def _(e):
                replay(e, "sync")

            @block.tensor
            def _(e):
                replay(e, "tensor")

            @block.vector
            def _(e):
                replay(e, "vector")

            @block.scalar
            def _(e):
                replay(e, "scalar")

            @block.gpsimd
            def _(e):
                replay(e, "gpsimd")


def build_program(dbg=False, upto=4, sub=99, nst_run=NST, sub2=99):
    nc = bass.Bass("TRN2", target_bir_lowering=False)

    def din(name, shape, dt=F32):
        return nc.dram_tensor(name, list(shape), dt, kind="ExternalInput").ap()

    def dscr(name, shape, dt):
        return nc.dram_tensor(name, list(shape), dt, kind=("ExternalOutput" if dbg else "Internal")).ap()

    x = din("x", [T, D])
    mem = din("mem", [NM, D])
    ln_in_g = din("ln_in_g", [D]); ln_in_b = din("ln_in_b", [D])
    ln_mem_g = din("ln_mem_g", [D]); ln_mem_b = din("ln_mem_b", [D])
    w_in = din("w_in", [D, 2560]); b_in_p = din("b_in_p", [128, 20]); b_v = din("b_v", [1, 512])
    w_dw_p = din("w_dw_p", [128, 4, 31]); b_dw_p = din("b_dw_p", [128, 4])
    lncg_p = din("lncg_p", [128, 4]); lncb_p = din("lncb_p", [128, 4])
    w_out = din("w_out", [D, D]); b_out = din("b_out", [1, D])
    ln1_g = din("ln1_g", [D]); ln1_b = din("ln1_b", [D])
    w_q = din("w_q", [D, D]); w_k = din("w_k", [D, D]); w_v = din("w_v", [D, D]); w_o = din("w_o", [D, D])
    ln2_g = din("ln2_g", [D]); ln2_b = din("ln2_b", [D])
    w_r = din("w_r", [D, NE]); b_r = din("b_r", [1, NE])
    w_gu = din("w_gu", [NE, D, 2048]); b_gu_p = din("b_gu_p", [128, NE, 16])
    w_dn = din("w_dn", [NE, D, D]); b_dn = din("b_dn", [NE, D])
    ln3_g = din("ln3_g", [D]); ln3_b = din("ln3_b", [D])
    out = nc.dram_tensor("out", [T, D], F32, kind="ExternalOutput").ap()

    h1s = dscr("h1s", [T, D], F32)
    h2s = dscr("h2s", [T, D], F32)
    Xe = dscr("Xe", [NSLOT, D], BF16)
    Ys = dscr("Ys", [NSLOT, D], F32)
    if dbg:
        catd = nc.dram_tensor("catd", [NST, 128, 8, 512], BF16, kind="ExternalOutput").ap()
        hd = nc.dram_tensor("hd", [NST, 128, 4, D], F32, kind="ExternalOutput").ap()

    with ExitStack() as es0:
        P = Prog(nc, es0)

        uid = [0]

        def sb(es, name, shape, dt):
            uid[0] += 1
            return es.enter_context(nc.sbuf_tensor("%s_%d" % (name, uid[0]), list(shape), dt))

        def ps(es, name, shape, dt):
            uid[0] += 1
            return es.enter_context(nc.psum_tensor("%s_%d" % (name, uid[0]), list(shape), dt))

        ident = sb(es0, "ident", [128, 128], BF16)
        ident_f = sb(es0, "ident_f", [128, 128], F32)
        ones_bf = sb(es0, "ones_bf", [128, 128], BF16)
        ones_f = sb(es0, "ones_f", [128, 128], F32)
        onesS = sb(es0, "onesS", [128, 128], F32)
        triInc = sb(es0, "triInc", [128, 128], BF16)
        maskD = sb(es0, "maskD", [128, 512], BF16)
        gates_all = sb(es0, "gates_all", [128, NT * 4], F32)
        dest_all = sb(es0, "dest_all", [128, NT * 4], I32)
        KmT = sb(es0, "KmT", [128, 8, NM], BF16)
        Vm = sb(es0, "Vm", [128, 2, D], BF16)
        CC = P.buf("constc"); CD = P.buf("constd")
        GD = P.buf("gd")
        DBGB = P.buf("dbg")

        def load_cast(src_ap, dst_ap, dstbuf, width, eng=None):
            P.dma("gpsimd", lambda e: e.dma_start(out=dst_ap, in_=src_ap), dstbuf, stream=True, war=True)

        def load_w(es, name, w_ap, kdim, ndim, wbuf):
            t = sb(es, name, [128, kdim // 128, ndim], BF16)
            wv = w_ap.rearrange("(c p) n -> p c n", p=128)
            per = min(kdim // 128, max(1, 2048 // ndim))
            if ndim > 2048:
                for c in range(kdim // 128):
                    for n0 in range(0, ndim, 1280):
                        load_cast(wv[:, c, n0:n0 + 1280], t[:, c, n0:n0 + 1280], wbuf, 1280)
            else:
                for c in range(0, kdim // 128, per):
                    load_cast(wv[:, c:c + per, :], t[:, c:c + per, :], wbuf, per * ndim)
            return t

        def bc_load(es, name, vec_ap, n=D):
            t = sb(es, name, [128, n], F32)
            P.dma("sync", lambda e: e.dma_start(out=t[:], in_=vec_ap.partition_broadcast(128)), CD, stream=True)
            return t

        def layer_norm_tile(*a, **k):
            for _ in ln_gen(*a, **k):
                pass

        def ln_gen(src, dst, g_t, b_t, srcB, dstB, tmpst, tmpB, n=D):
            st_t, mv_t, rs_t = tmpst
            nch = n // 512
            srcBs = list(srcB) if isinstance(srcB, (list, tuple)) else [srcB]
            yield P.group("vector", [(lambda e, c=c: e.bn_stats(out=st_t[:, c * 6:(c + 1) * 6], in_=src[:, c * 512:(c + 1) * 512]))
                               for c in range(nch)], reads=srcBs, writes=[tmpB])
            yield P.op("vector", lambda e: e.bn_aggr(out=mv_t[:], in_=st_t[:, 0:nch * 6]), reads=[tmpB], writes=[tmpB])
            yield P.op("scalar", lambda e: e.activation(out=rs_t[:, 0:1], in_=mv_t[:, 1:2], func=AF.Ln, bias=EPS_T[:, 0:1]),
                 reads=[tmpB, CC, CD], writes=[tmpB])
            yield P.op("scalar", lambda e: e.activation(out=rs_t[:, 1:2], in_=rs_t[:, 0:1], func=AF.Exp, scale=-0.5),
                 reads=[tmpB], writes=[tmpB])
            yield P.op("vector", lambda e: e.tensor_scalar(out=dst, in0=src, scalar1=mv_t[:, 0:1], scalar2=rs_t[:, 1:2],
                                                     op0=ALU.subtract, op1=ALU.mult),
                 reads=srcBs + [tmpB], writes=[dstB])
            yield P.op("gpsimd", lambda e: e.tensor_tensor(out=dst, in0=dst, in1=g_t[:, 0:n], op=ALU.mult),
                 reads=[dstB, CC, CD], writes=[dstB])
            yield P.op("gpsimd", lambda e: e.tensor_tensor(out=dst, in0=dst, in1=b_t[:, 0:n], op=ALU.add),
                 reads=[dstB, CC, CD], writes=[dstB])

        EPS_T = sb(es0, "eps_t", [128, 1], F32)
        P.op("gpsimd", lambda e: e.memset(EPS_T[:], EPS), writes=[CC])
        ONE_T = sb(es0, "one_t", [128, 1], F32)
        P.op("gpsimd", lambda e: e.memset(ONE_T[:], 1.0), writes=[CC])

        def transpose_to(src_bf, srcB, dstT_ap, dstB, pT, pTB, nchunks=8, eng="scalar"):
            P.group("tensor", [(lambda e, c=c: e.transpose(out=pT[:, c * 128:(c + 1) * 128],
                                                           in_=src_bf[:, c * 128:(c + 1) * 128], identity=ident[:]))
                               for c in range(nchunks)], reads=[srcB, CC, CD], writes=[pTB])
            pv = pT[:, 0:nchunks * 128].rearrange("p (c t) -> p c t", c=nchunks)
            if eng == "scalar":
                P.op("scalar", lambda e: e.copy(out=dstT_ap, in_=pv), reads=[pTB], writes=[dstB])
            else:
                P.op(eng, lambda e: e.tensor_copy(out=dstT_ap, in_=pv), reads=[pTB], writes=[dstB])

        with ExitStack() as est:
            tmpf = sb(est, "tmpf", [128, 512], F32)
            P.op("gpsimd", lambda e: e.memset(ones_f[:], 1.0), writes=[CC])
            P.op("gpsimd", lambda e: e.memset(onesS[:], 1.0 / 512.0), writes=[CC])
            P.op("gpsimd", lambda e: e.memset(tmpf[:], 1.0), writes=[CC])
            P.op("vector", lambda e: e.tensor_copy(out=ones_bf[:], in_=ones_f[:]), reads=[CC, CD], writes=[CC])
            tA = sb(est, "tA", [128, 128], F32)
            P.op("gpsimd", lambda e: e.affine_select(out=tA[:], in_=ones_f[:], pattern=[[1, 128]],
                                                     compare_op=ALU.is_equal, fill=0.0, base=0,
                                                     channel_multiplier=-1), reads=[CC, CD], writes=[CC])
            P.op("vector", lambda e: e.tensor_copy(out=ident[:], in_=tA[:]), reads=[CC, CD], writes=[CC])
            P.op("vector", lambda e: e.tensor_copy(out=ident_f[:], in_=tA[:]), reads=[CC, CD], writes=[CC])
            tB = sb(est, "tB", [128, 128], F32)
            P.op("gpsimd", lambda e: e.affine_select(out=tB[:], in_=ones_f[:], pattern=[[-1, 128]],
                                                     compare_op=ALU.is_ge, fill=0.0, base=0,
                                                     channel_multiplier=1), reads=[CC, CD], writes=[CC])
            P.op("vector", lambda e: e.tensor_copy(out=triInc[:], in_=tB[:]), reads=[CC, CD], writes=[CC])
            tC = sb(est, "tC", [128, 512], F32)
            P.op("gpsimd", lambda e: e.affine_select(out=tC[:], in_=tmpf[:], pattern=[[0, 4], [1, 128]],
                                                     compare_op=ALU.is_gt, fill=0.0, base=0,
                                                     channel_multiplier=-1), reads=[CC, CD], writes=[CC])
            P.op("vector", lambda e: e.tensor_copy(out=maskD[:], in_=tC[:]), reads=[CC, CD], writes=[CC])
            P.barrier()
            P.flush()

        H1S = [P.buf("h1s%d" % i) for i in range(2)]; H2S = [P.buf("h2s%d" % i) for i in range(2)]
        XE = [P.buf("xes%d" % i) for i in range(2)]; YS = [P.buf("ys%d" % i) for i in range(2)]
        OUTB = [P.buf("out%d" % i) for i in range(2)]

        with ExitStack() as es:
            g_in = bc_load(es, "g_in", ln_in_g); bb_in = bc_load(es, "bb_in", ln_in_b)
            g_1 = bc_load(es, "g_1", ln1_g); bb_1 = bc_load(es, "bb_1", ln1_b)
            binp = sb(es, "binp", [128, 20], F32)
            wdwp = sb(es, "wdwp", [128, 4, 31], F32)
            bdwp = sb(es, "bdwp", [128, 4], F32)
            lcg = sb(es, "lcg", [128, 4], F32)
            lcb = sb(es, "lcb", [128, 4], F32)
            for t_, a_ in [(binp, b_in_p), (wdwp, w_dw_p), (bdwp, b_dw_p), (lcg, lncg_p), (lcb, lncb_p)]:
                P.dma("sync", lambda e, t_=t_, a_=a_: e.dma_start(out=t_[:], in_=a_), CD, stream=True)
            brow_f = sb(es, "brow_f", [1, 1536], F32)
            brow = sb(es, "brow", [1, 1536], BF16)
            P.dma("sync", lambda e: e.dma_start(out=brow_f[:, 0:512], in_=b_v), CD, stream=True)
            P.dma("sync", lambda e: e.dma_start(out=brow_f[:, 512:1536], in_=b_out), CD, stream=True)
            P.op("vector", lambda e: e.tensor_copy(out=brow[:], in_=brow_f[:]), reads=[CC, CD], writes=[CC])

            WIN = P.buf("w_in"); WOUT = P.buf("w_out")
            w_in_bf = load_w(es, "w_in_bf", w_in, D, 2560, WIN)
            w_out_bf = load_w(es, "w_out_bf", w_out, D, D, WOUT)

            st_t = sb(es, "st_t", [128, 12], F32); mv_t = sb(es, "mv_t", [128, 2], F32); rs_t = sb(es, "rs_t", [128, 2], F32)
            LNT = P.buf("lnt")
            lnt = (st_t, mv_t, rs_t)

            pT = [ps(es, "pT%d" % i, [128, 1024], BF16) for i in range(2)]
            pTB = [P.buf() for _ in range(2)]
            pmm = [ps(es, "pmm%d" % i, [128, 512], F32) for i in range(2)]
            pmmB = [P.buf() for _ in range(2)]
            pz = ps(es, "pz", [128, 512], F32); pzB = P.buf()
            pz2 = pmm[0]; pz2B = pmmB[0]
            pst2 = pmm[1]; pst2B = pmmB[1]
            pc = ps(es, "pc", [128, 512], F32); pcB = P.buf()
            po = ps(es, "po", [128, 2, 2, 128], F32); poB = P.buf()
            pst = ps(es, "pst", [128, 512], F32); pstB = P.buf()
            mmi = [0]

            def next_pmm():
                i = mmi[0] % 2
                mmi[0] += 1
                return pmm[i], pmmB[i]

            tpi = [0]

            def next_pT():
                i = tpi[0] % 2
                tpi[0] += 1
                return pT[i], pTB[i]

            hbs = [sb(es, "hb%d" % i, [128, D], BF16) for i in range(4)]; hbsB = [P.buf() for _ in range(4)]
            hb = hbs[0]; hbB = hbsB[0]
            lnts = [lnt] + [(sb(es, "st_t%d" % i, [128, 12], F32), sb(es, "mv_t%d" % i, [128, 2], F32), sb(es, "rs_t%d" % i, [128, 2], F32))
                            for i in range(3)]
            LNTs = [LNT] + [P.buf() for _ in range(3)]

            with ExitStack() as esm:
                g_m = bc_load(esm, "g_m", ln_mem_g); bb_m = bc_load(esm, "bb_m", ln_mem_b)
                xt = [sb(esm, "xt%d" % i, [128, D], F32) for i in range(2)]
                xtB = [P.buf("xt%d" % i) for i in range(2)]
                WK = P.buf("wk"); WV = P.buf("wv")
                wk_bf = load_w(esm, "wk_bf", w_k, D, D, WK)
                wv_bf = load_w(esm, "wv_bf", w_v, D, D, WV)
                memT = sb(esm, "memT", [128, 8, NM], BF16); memTB = P.buf()
                KMT = P.buf("kmt"); VM = P.buf("vm")
                mn = sb(esm, "mn", [128, D], F32); mnB = P.buf()
                for mt in range(2):
                    P.dma("sync", lambda e, mt=mt: e.dma_start(out=xt[mt][:], in_=mem[mt * 128:(mt + 1) * 128, :]), xtB[mt])
                    layer_norm_tile(xt[mt][:], mn[:], g_m, bb_m, xtB[mt], mnB, lnt, LNT)
                    P.op("scalar", lambda e: e.copy(out=hb[:], in_=mn[:]), reads=[mnB], writes=[hbB])
                    p_, pB_ = next_pT()
                    transpose_to(hb, hbB, memT[:, :, mt * 128:(mt + 1) * 128], memTB, p_, pB_)
                for oc in range(8):
                    p_, pB_ = next_pmm()
                    P.group("tensor", [(lambda e, kc=kc, oc=oc, p_=p_: e.matmul(
                        p_[:, 0:NM], lhsT=wk_bf[:, kc, oc * 128:(oc + 1) * 128], rhs=memT[:, kc, :],
                        start=(kc == 0), stop=(kc == 7))) for kc in range(8)], reads=[WK, memTB], writes=[pB_])
                    P.op("vector", lambda e, oc=oc, p_=p_: e.tensor_copy(out=KmT[:, oc, :], in_=p_[:, 0:NM]),
                         reads=[pB_], writes=[KMT])
                for mt in range(2):
                    for hf in range(2):
                        p_, pB_ = next_pmm()
                        P.group("tensor", [(lambda e, kc=kc, mt=mt, hf=hf, p_=p_: e.matmul(
                            p_[:, :], lhsT=memT[:, kc, mt * 128:(mt + 1) * 128], rhs=wv_bf[:, kc, hf * 512:(hf + 1) * 512],
                            start=(kc == 0), stop=(kc == 7))) for kc in range(8)], reads=[WV, memTB], writes=[pB_])
                        P.op("vector", lambda e, mt=mt, hf=hf, p_=p_: e.tensor_copy(out=Vm[:, mt, hf * 512:(hf + 1) * 512], in_=p_[:, :]),
                             reads=[pB_], writes=[VM])
                P.barrier()
                P.flush()

            if upto < 1:
                return nc
            h_res = sb(es, "h_res", [128, 4, D], F32); hresB = [P.buf("hres%d" % i) for i in range(4)]
            hT = sb(es, "hT", [128, 8, 512], BF16); hTB = P.buf()
            uwin = sb(es, "uwin", [128, 4, PADL + 512], F32); uB = P.buf()
            sg = sb(es, "sg", [128, 512], F32); sgB = P.buf()
            acc = sb(es, "acc", [128, 2048], F32); accB = [P.buf() for _ in range(4)]
            m_sb = sb(es, "m_sb", [128, 512], F32); msB = P.buf()
            rstd_c = sb(es, "rstd_c", [128, 512], F32); rcB = P.buf()
            qTm = [sb(es, "qTm%d" % i, [128, 4, 512], BF16) for i in range(2)]; qTB = P.buf()
            kT = sb(es, "kT", [128, 4, 640], BF16); kTB = P.buf()
            kTn = sb(es, "kTn", [128, 4, 640], BF16); kTnB = P.buf()
            vwin = sb(es, "vwin", [128, 5, 512], BF16); vB = P.buf()
            e1 = sb(es, "e1", [128, 512], F32); e1B = P.buf()
            sq = sb(es, "sq", [128, 512], F32); sqB = P.buf()
            spm = [sb(es, "spm%d" % i, [128, 512], BF16) for i in range(2)]; spmB = [P.buf() for _ in range(2)]
            spp = sb(es, "spp", [128, 512], BF16); sppB = P.buf()
            e1b = sb(es, "e1b", [128, 512], F32); e1bB = P.buf()
            wtmp = sb(es, "wtmp", [128, 512], BF16); wtB = P.buf()
            wsm = [sb(es, "wsm%d" % i, [128, 512], BF16) for i in range(2)]; wmB = [P.buf() for _ in range(2)]
            wsp = [sb(es, "wsp%d" % i, [128, 512], BF16) for i in range(2)]; wpB = [P.buf() for _ in range(2)]
            catT = sb(es, "catT", [128, 8, 512], BF16); catB = P.buf()
            h1t = [sb(es, "h1t%d" % i, [128, D], F32) for i in range(2)]; h1tB = [P.buf("h1t%d" % i) for i in range(2)]

            P.op("vector", lambda e: e.memset(qTm[0][:], 0.0), writes=[qTB])
            P.op("vector", lambda e: e.memset(qTm[1][:], 0.0), writes=[qTB])
            P.op("vector", lambda e: e.memset(uwin[:], 0.0), writes=[uB])
            P.op("vector", lambda e: e.memset(kT[:], 0.0), writes=[kTB])
            P.op("vector", lambda e: e.memset(kTn[:], 0.0), writes=[kTnB])
            P.op("vector", lambda e: e.memset(vwin[:], 0.0), writes=[vB])

            def run_il(gens):
                gens = list(gens)
                while gens:
                    for g in list(gens):
                        try:
                            next(g)
                        except StopIteration:
                            gens.remove(g)

            def tile_in_gen(st, j):
                ti = st * 4 + j
                yield P.dma("sync", lambda e: e.dma_start(out=h_res[:, j, :], in_=x[ti * 128:(ti + 1) * 128, :]), hresB[j])
                yield from ln_gen(h_res[:, j, :], h_res[:, j, :], g_in, bb_in, hresB[j], hresB[j], lnts[j], LNTs[j])
                yield P.op("scalar", lambda e: e.copy(out=hbs[j][:], in_=h_res[:, j, :]), reads=[hresB[j]], writes=[hbsB[j]])
                p_, pB_ = next_pT()
                transpose_to(hbs[j], hbsB[j], hT[:, :, j * 128:(j + 1) * 128], hTB, p_, pB_)
                yield

            def conv_gen(st):
                for c in range(4):
                    cs = slice(c * 512, (c + 1) * 512)
                    yield P.op("vector", lambda e, c=c, cs=cs: e.tensor_scalar(
                        out=acc[:, cs], in0=uwin[:, c, 0:512], scalar1=wdwp[:, c, 0:1],
                        scalar2=bdwp[:, c:c + 1], op0=ALU.mult, op1=ALU.add), reads=[uB, CC, CD], writes=[accB[c]])
                    for jt in range(1, 31):
                        yield P.op("vector", lambda e, c=c, cs=cs, jt=jt: e.scalar_tensor_tensor(
                            out=acc[:, cs], in0=uwin[:, c, jt:jt + 512], scalar=wdwp[:, c, jt:jt + 1], in1=acc[:, cs],
                            op0=ALU.mult, op1=ALU.add), reads=[uB, CC, CD, accB[c]], writes=[accB[c]])
                yield P.group("tensor", [(lambda e, c=c: e.matmul(pst[:, :], lhsT=onesS[:], rhs=acc[:, c * 512:(c + 1) * 512],
                                                                  start=(c == 0), stop=(c == 3))) for c in range(4)],
                              reads=accB + [CC, CD], writes=[pstB])
                yield P.op("scalar", lambda e: e.copy(out=m_sb[:], in_=pst[:, :]), reads=[pstB], writes=[msB])
                for c in range(4):
                    cs = slice(c * 512, (c + 1) * 512)
                    yield P.op("gpsimd", lambda e, cs=cs: e.tensor_tensor(out=sq[:], in0=acc[:, cs], in1=acc[:, cs], op=ALU.mult),
                               reads=[accB[c]], writes=[sqB])
                    yield P.op("tensor", lambda e, c=c: e.matmul(pst2[:, :], lhsT=onesS[:], rhs=sq[:], start=(c == 0), stop=(c == 3)),
                               reads=[sqB, CC, CD], writes=[pst2B])
                yield P.op("gpsimd", lambda e: e.tensor_tensor(out=sq[:], in0=m_sb[:], in1=m_sb[:], op=ALU.mult), reads=[msB], writes=[sqB])
                yield P.op("vector", lambda e: e.tensor_tensor(out=rstd_c[:], in0=pst2[:, :], in1=sq[:], op=ALU.subtract),
                           reads=[pst2B, sqB], writes=[rcB])
                yield P.op("scalar", lambda e: e.activation(out=rstd_c[:], in_=rstd_c[:], func=AF.Ln, bias=EPS_T[:, 0:1]),
                           reads=[rcB, CC, CD], writes=[rcB])
                yield P.op("scalar", lambda e: e.activation(out=rstd_c[:], in_=rstd_c[:], func=AF.Exp, scale=-0.5), reads=[rcB], writes=[rcB])
                for c in range(4):
                    cs = slice(c * 512, (c + 1) * 512)
                    yield P.op("gpsimd", lambda e, cs=cs: e.tensor_tensor(out=acc[:, cs], in0=acc[:, cs], in1=m_sb[:], op=ALU.subtract),
                               reads=[accB[c], msB], writes=[accB[c]])
                    yield P.op("vector", lambda e, cs=cs: e.tensor_tensor(out=acc[:, cs], in0=acc[:, cs], in1=rstd_c[:], op=ALU.mult),
                               reads=[accB[c], rcB], writes=[accB[c]])
                    yield P.op("scalar", lambda e, c=c, cs=cs: e.activation(out=catT[:, c, :], in_=acc[:, cs], func=AF.Silu,
                                                                            scale=lcg[:, c:c + 1], bias=lcb[:, c:c + 1]),
                               reads=[accB[c], CC, CD], writes=[catB])
                yield P.op("vector", lambda e: e.tensor_copy(out=uwin[:, :, 0:PADL], in_=uwin[:, :, 512:512 + PADL]),
                           reads=[uB] + accB, writes=[uB])

            def sb_gen(st):
                ui = 0
                for jq in range(4):
                    qi = st * 4 + jq
                    blocks = [1 + jq] + ([jq] if qi > 0 else [])
                    nb = len(blocks)
                    for hg in range(2):
                        s2 = ui % 2
                        ui += 1
                        wsm_, wmB_ = wsm[s2], wmB[s2]
                        wsp_, wpB_ = wsp[s2], wpB[s2]
                        spd_, spdB_ = spm[s2], spmB[s2]
                        for bi, blk in enumerate(blocks):
                            diag = (bi == 0)
                            kc0 = blk * 128
                            pz_, pzB_ = (pz, pzB) if diag else (pz2, pz2B)
                            fz = []
                            for hh in range(4):
                                h = hg * 4 + hh
                                c = h // 2
                                fz.append(lambda e, hh=hh, c=c, h=h, kc0=kc0, jq=jq, pz_=pz_: e.matmul(
                                    pz_[:, hh * 128:(hh + 1) * 128], lhsT=kT[:, c, kc0:kc0 + 128],
                                    rhs=qTm[h % 2][:, c, jq * 128:(jq + 1) * 128], start=True, stop=True))
                            yield P.group("tensor", fz, reads=[kTB, qTB], writes=[pzB_])
                            e1_, e1B_ = (e1, e1B) if diag else (e1b, e1bB)
                            yield P.op("scalar", lambda e, e1_=e1_, pz_=pz_: e.activation(out=e1_[:], in_=pz_[:, :], func=AF.Exp, scale=0.125),
                                       reads=[pzB_], writes=[e1B_])
                            if diag:
                                yield P.op("scalar", lambda e: e.activation(out=e1[:], in_=e1[:], func=AF.Ln, bias=ONE_T[:, 0:1]),
                                           reads=[e1B, CC, CD], writes=[e1B])
                                yield P.op("vector", lambda e, spd_=spd_: e.tensor_tensor(out=spd_[:], in0=e1[:], in1=maskD[:], op=ALU.mult),
                                           reads=[e1B, CC, CD], writes=[spdB_])
                                cur, curB = spd_, spdB_
                            else:
                                yield P.op("scalar", lambda e: e.activation(out=spp[:], in_=e1b[:], func=AF.Ln, bias=ONE_T[:, 0:1]),
                                           reads=[e1bB, CC, CD], writes=[sppB])
                                cur, curB = spp, sppB
                            fc = []
                            for hh in range(4):
                                h = hg * 4 + hh
                                c = h // 2
                                sl = slice(hh * 128, (hh + 1) * 128)
                                fc.append(lambda e, sl=sl, cur=cur: e.matmul(pc[:, sl], lhsT=triInc[:], rhs=cur[:, sl],
                                                                             start=True, stop=False))
                                if not diag:
                                    fc.append(lambda e, sl=sl, spd_=spd_: e.matmul(pc[:, sl], lhsT=ones_bf[:], rhs=spd_[:, sl],
                                                                                   start=False, stop=False))
                                fc.append(lambda e, sl=sl, c=c, h=h, kc0=kc0, jq=jq: e.matmul(
                                    pc[:, sl], lhsT=kTn[:, c, kc0:kc0 + 128],
                                    rhs=qTm[h % 2][:, c, jq * 128:(jq + 1) * 128], start=False, stop=True))
                            yield P.group("tensor", fc, reads=[curB, spdB_, kTnB, qTB, CC, CD], writes=[pcB])
                            if diag:
                                yield P.op("scalar", lambda e: e.activation(out=wtmp[:], in_=pc[:, :], func=AF.Exp, scale=-1.0),
                                           reads=[pcB], writes=[wtB])
                                yield P.op("vector", lambda e, wsm_=wsm_: e.tensor_tensor(out=wsm_[:], in0=wtmp[:], in1=maskD[:], op=ALU.mult),
                                           reads=[wtB, CC, CD], writes=[wmB_])
                            else:
                                yield P.op("scalar", lambda e, wsp_=wsp_: e.activation(out=wsp_[:], in_=pc[:, :], func=AF.Exp, scale=-1.0),
                                           reads=[pcB], writes=[wpB_])
                        fo = []
                        for hh in range(4):
                            h = hg * 4 + hh
                            cl = hh // 2
                            fo.append(lambda e, hh=hh, h=h, cl=cl, jq=jq, nb=nb, wsm_=wsm_: e.matmul(
                                po[:, hh % 2, cl, :], lhsT=vwin[:, 1 + jq, (h // 2) * 128:(h // 2 + 1) * 128],
                                rhs=wsm_[:, hh * 128:(hh + 1) * 128], start=True, stop=(nb == 1)))
                            if nb == 2:
                                fo.append(lambda e, hh=hh, h=h, cl=cl, jq=jq, wsp_=wsp_: e.matmul(
                                    po[:, hh % 2, cl, :], lhsT=vwin[:, jq, (h // 2) * 128:(h // 2 + 1) * 128],
                                    rhs=wsp_[:, hh * 128:(hh + 1) * 128], start=False, stop=True))
                        yield P.group("tensor", fo, reads=[vB, wmB_] + ([wpB_] if nb == 2 else []), writes=[poB])
                        yield P.op("scalar", lambda e, hg=hg, jq=jq: e.copy(
                            out=catT[0:64, 4 + hg * 2:6 + hg * 2, jq * 128:(jq + 1) * 128], in_=po[0:64, 0, :, :]),
                            reads=[poB], writes=[catB])
                        yield P.op("vector", lambda e, hg=hg, jq=jq: e.tensor_copy(
                            out=catT[64:128, 4 + hg * 2:6 + hg * 2, jq * 128:(jq + 1) * 128], in_=po[64:128, 1, :, :]),
                            reads=[poB], writes=[catB])
                yield P.op("gpsimd", lambda e: e.tensor_copy(out=kT[:, :, 0:128], in_=kT[:, :, 512:640]), reads=[kTB], writes=[kTB])
                yield P.op("gpsimd", lambda e: e.tensor_copy(out=kTn[:, :, 0:128], in_=kTn[:, :, 512:640]), reads=[kTnB], writes=[kTnB])
                yield P.op("gpsimd", lambda e: e.tensor_copy(out=vwin[:, 0, :], in_=vwin[:, 4, :]), reads=[vB], writes=[vB])

            def outproj_gen(st, j):
                ti = st * 4 + j
                pj = j % 2
                for hf in range(2):
                    p_, pB_ = next_pmm()
                    fns = [(lambda e, kc=kc, p_=p_, hf=hf: e.matmul(
                        p_[:, :], lhsT=catT[:, kc, j * 128:(j + 1) * 128], rhs=w_out_bf[:, kc, hf * 512:(hf + 1) * 512],
                        start=(kc == 0), stop=False)) for kc in range(8)]
                    fns.append(lambda e, p_=p_, hf=hf: e.matmul(p_[:, :], lhsT=ones_bf[0:1, :],
                                                         rhs=brow[0:1, 512 + hf * 512:512 + (hf + 1) * 512],
                                                         start=False, stop=True))
                    yield P.group("tensor", fns, reads=[WOUT, catB, CC, CD], writes=[pB_])
                    yield P.op("vector", lambda e, hf=hf, p_=p_: e.scalar_tensor_tensor(
                        out=acc[:, pj * 1024 + hf * 512:pj * 1024 + (hf + 1) * 512], in0=h_res[:, j, hf * 512:(hf + 1) * 512], scalar=ALPHA,
                        in1=p_[:, :], op0=ALU.mult, op1=ALU.add), reads=[hresB[j], pB_], writes=[accB[pj * 2 + hf]])
                yield from ln_gen(acc[:, pj * 1024:(pj + 1) * 1024], h1t[pj][:], g_1, bb_1, accB[pj * 2:pj * 2 + 2], h1tB[pj], lnts[j], LNTs[j])
                yield P.dma("gpsimd", lambda e: e.dma_start(out=h1s[ti * 128:(ti + 1) * 128, :], in_=h1t[pj][:]),
                            H1S[pj], reads=[h1tB[pj]], stream=True)

            for st in range(nst_run):
                run_il([tile_in_gen(st, j) for j in range(4)])
                for c in range(4):
                    p_, pB_ = next_pmm()
                    oc = 4 + c
                    P.group("tensor", [(lambda e, kc=kc, oc=oc, p_=p_: e.matmul(
                        p_[:, :], lhsT=w_in_bf[:, kc, oc * 128:(oc + 1) * 128], rhs=hT[:, kc, :],
                        start=(kc == 0), stop=(kc == 7))) for kc in range(8)], reads=[WIN, hTB], writes=[pB_])
                    P.op("scalar", lambda e, oc=oc, p_=p_: e.activation(out=sg[:], in_=p_[:, :], func=AF.Sigmoid,
                                                                          bias=binp[:, oc:oc + 1]),
                         reads=[pB_, CC, CD], writes=[sgB])
                    p2, pB2 = next_pmm()
                    oc2 = c
                    P.group("tensor", [(lambda e, kc=kc, oc2=oc2, p2=p2: e.matmul(
                        p2[:, :], lhsT=w_in_bf[:, kc, oc2 * 128:(oc2 + 1) * 128], rhs=hT[:, kc, :],
                        start=(kc == 0), stop=(kc == 7))) for kc in range(8)], reads=[WIN, hTB], writes=[pB2])
                    P.op("vector", lambda e, c=c, p2=p2: e.scalar_tensor_tensor(
                        out=uwin[:, c, PADL:PADL + 512], in0=p2[:, :], scalar=binp[:, c:c + 1], in1=sg[:],
                        op0=ALU.add, op1=ALU.mult), reads=[pB2, sgB, CC, CD], writes=[uB])
                for c in range(4):
                    p_, pB_ = next_pmm()
                    oc = 8 + c
                    P.group("tensor", [(lambda e, kc=kc, oc=oc, p_=p_: e.matmul(
                        p_[:, :], lhsT=w_in_bf[:, kc, oc * 128:(oc + 1) * 128], rhs=hT[:, kc, :],
                        start=(kc == 0), stop=(kc == 7))) for kc in range(8)], reads=[WIN, hTB], writes=[pB_])
                    P.op("scalar", lambda e, oc=oc, c=c, p_=p_: e.activation(out=qTm[0][0:64, c, :], in_=p_[0:64, :], func=AF.Identity,
                                                                               bias=binp[0:64, oc:oc + 1]),
                         reads=[pB_, CC, CD], writes=[qTB])
                    P.op("scalar", lambda e, oc=oc, c=c, p_=p_: e.activation(out=qTm[1][64:128, c, :], in_=p_[64:128, :], func=AF.Identity,
                                                                               bias=binp[64:128, oc:oc + 1]),
                         reads=[pB_, CC, CD], writes=[qTB])
                for c in range(4):
                    p_, pB_ = next_pmm()
                    oc = 12 + c
                    P.group("tensor", [(lambda e, kc=kc, oc=oc, p_=p_: e.matmul(
                        p_[:, :], lhsT=w_in_bf[:, kc, oc * 128:(oc + 1) * 128], rhs=hT[:, kc, :],
                        start=(kc == 0), stop=(kc == 7))) for kc in range(8)], reads=[WIN, hTB], writes=[pB_])
                    P.op("vector", lambda e, oc=oc, c=c, p_=p_: e.tensor_scalar(
                        out=kT[:, c, 128:640], in0=p_[:, :], scalar1=binp[:, oc:oc + 1], scalar2=None, op0=ALU.add),
                         reads=[pB_, CC, CD], writes=[kTB])
                    P.op("vector", lambda e, oc=oc, c=c, p_=p_: e.tensor_scalar(
                        out=kTn[:, c, 128:640], in0=p_[:, :], scalar1=binp[:, oc:oc + 1], scalar2=-0.125,
                        op0=ALU.add, op1=ALU.mult), reads=[pB_, CC, CD], writes=[kTnB])
                for j in range(4):
                    p_, pB_ = next_pmm()
                    fns = [(lambda e, kc=kc, j=j, p_=p_: e.matmul(
                        p_[:, :], lhsT=hT[:, kc, j * 128:(j + 1) * 128], rhs=w_in_bf[:, kc, 2048:2560],
                        start=(kc == 0), stop=False)) for kc in range(8)]
                    fns.append(lambda e, p_=p_: e.matmul(p_[:, :], lhsT=ones_bf[0:1, :], rhs=brow[0:1, 0:512],
                                                          start=False, stop=True))
                    P.group("tensor", fns, reads=[WIN, hTB, CC, CD], writes=[pB_])
                    P.op("scalar", lambda e, j=j, p_=p_: e.copy(out=vwin[:, 1 + j, :], in_=p_[:, :]), reads=[pB_], writes=[vB])
                run_il([conv_gen(st), sb_gen(st)])
                if dbg:
                    P.dma("sync", lambda e, st=st: e.dma_start(out=catd[st], in_=catT[:]), DBGB, reads=[catB], stream=True)
                    P.dma("sync", lambda e, st=st: e.dma_start(out=hd[st], in_=h_res[:]), DBGB, reads=hresB, stream=True)
                run_il([outproj_gen(st, j) for j in range(2)])
                run_il([outproj_gen(st, j) for j in range(2, 4)])
            P.barrier()
            P.flush()

        with ExitStack() as es:
          if upto >= 2:
            g_2 = bc_load(es, "g_2", ln2_g); bb_2 = bc_load(es, "bb_2", ln2_b)
            WQ = P.buf("wq"); WO = P.buf("wo"); WR = P.buf("wr")
            wq_bf = load_w(es, "wq_bf", w_q, D, D, WQ)
            wo_bf = load_w(es, "wo_bf", w_o, D, D, WO)
            wr_f = sb(es, "wr_f", [128, 8, NE], F32)
            P.dma("sync", lambda e: e.dma_start(out=wr_f[:], in_=w_r.rearrange("(c p) n -> p c n", p=128)), CD, stream=True)
            brr_f = sb(es, "brr_f", [1, NE], F32); brr = sb(es, "brr", [1, NE], BF16)
            P.dma("sync", lambda e: e.dma_start(out=brr_f[:], in_=b_r), CD, stream=True)
            P.op("vector", lambda e: e.tensor_copy(out=brr[:], in_=brr_f[:]), reads=[CC, CD], writes=[CC])
            eoff = sb(es, "eoff", [128, NE], F32)
            eoff_i = sb(es, "eoff_i", [128, NE], I32)
            P.op("gpsimd", lambda e: e.iota(eoff_i[:], pattern=[[CAP, NE]], base=0, channel_multiplier=0), writes=[CC])
            P.op("vector", lambda e: e.tensor_copy(out=eoff[:], in_=eoff_i[:]), reads=[CC, CD], writes=[CC])

            st_t = sb(es, "st_t", [128, 12], F32); mv_t = sb(es, "mv_t", [128, 2], F32); rs_t = sb(es, "rs_t", [128, 2], F32)
            LNT = P.buf("lntB")
            lnt = (st_t, mv_t, rs_t)
            pT = [ps(es, "pT%d" % i, [128, 1024], BF16) for i in range(2)]
            pTB = [P.buf() for _ in range(2)]
            pmm = [ps(es, "pmm%d" % i, [128, 512], F32) for i in range(2)]
            pmmB = [P.buf() for _ in range(2)]
            pS = [ps(es, "pS%d" % i, [128, 512], F32) for i in range(2)]
            pSB = [P.buf() for _ in range(2)]
            pSum = ps(es, "pSum", [128, 512], F32); pSumB = P.buf()
            pO = ps(es, "pO", [128, 512], F32); pOB = P.buf()
            mmi = [0]; tpi = [0]; psi = [0]

            def next_pmm():
                i = mmi[0] % 2
                mmi[0] += 1
                return pmm[i], pmmB[i]

            def next_pT():
                i = tpi[0] % 2
                tpi[0] += 1
                return pT[i], pTB[i]

            h1r = sb(es, "h1r", [128, 4, D], F32); h1rB = [P.buf("h1r%d" % j) for j in range(4)]
            hb = sb(es, "hbB", [128, D], BF16); hbB = P.buf()
            h1T = sb(es, "h1T", [128, 8, 512], BF16); h1TB = P.buf()
            qmT = sb(es, "qmT", [128, 8, 512], BF16); qmTB = P.buf()
            Et = [sb(es, "Et%d" % i, [128, 512], BF16) for i in range(2)]; EtB = [P.buf() for _ in range(2)]
            rinv = sb(es, "rinv", [128, 512], F32); rinvB = P.buf()
            OT = sb(es, "OT", [128, 8, 512], BF16); OTB = P.buf()
            pre = sb(es, "preB", [128, D], F32); preB = P.buf()
            h2t = [sb(es, "h2t%d" % i, [128, D], F32) for i in range(2)]; h2tB = [P.buf("h2t%d" % i) for i in range(2)]
            h2b = [sb(es, "h2b%d" % i, [128, D], BF16) for i in range(2)]; h2bB = [P.buf("h2b%d" % i) for i in range(2)]
            h2T = sb(es, "h2T", [128, 8, 128], F32); h2TB = P.buf()
            lg = sb(es, "lg", [128, NE], F32); lgB = P.buf()
            m8 = sb(es, "m8", [128, 8], F32); m8B = P.buf()
            nm = sb(es, "nm", [128, 1], F32)
            msk = sb(es, "msk", [128, NE], F32); mskB = P.buf()
            mskb = sb(es, "mskb", [128, NE], BF16); mskbB = P.buf()
            ex = sb(es, "ex", [128, NE], F32); exB = P.buf()
            ssum = sb(es, "ssum", [128, 2], F32)
            G = sb(es, "G", [128, NE], F32); GB = P.buf()
            cum = sb(es, "cum", [128, NE], F32); cumB = P.buf()
            cumb = sb(es, "cumb", [128, NE], BF16); cumbB = P.buf()
            posf = sb(es, "posf", [128, NE], F32); posB = P.buf()
            oh = sb(es, "oh", [128, NE], F32); ohB = P.buf()
            tmp32 = sb(es, "tmp32", [128, NE], F32); t32B = P.buf()
            dstf = sb(es, "dstf", [128, 4], F32); dstfB = P.buf()
            P.op("vector", lambda e: e.memset(cum[:], 0.0), writes=[cumB])
            P.op("vector", lambda e: e.memset(cumb[:], 0.0), writes=[cumbB])

            for st in range(NST):
                for j in range(4):
                    ti = st * 4 + j
                    P.dma("sync", lambda e, ti=ti, j=j: e.dma_start(out=h1r[:, j, :], in_=h1s[ti * 128:(ti + 1) * 128, :]),
                          h1rB[j], reads=H1S)
                    P.op("scalar", lambda e, j=j: e.copy(out=hb[:], in_=h1r[:, j, :]), reads=[h1rB[j]], writes=[hbB])
                    p_, pB_ = next_pT()
                    transpose_to(hb, hbB, h1T[:, :, j * 128:(j + 1) * 128], h1TB, p_, pB_, eng="vector")
                for oc in range(8):
                    p_, pB_ = next_pmm()
                    P.group("tensor", [(lambda e, kc=kc, oc=oc, p_=p_: e.matmul(
                        p_[:, :], lhsT=wq_bf[:, kc, oc * 128:(oc + 1) * 128], rhs=h1T[:, kc, :],
                        start=(kc == 0), stop=(kc == 7))) for kc in range(8)], reads=[WQ, h1TB], writes=[pB_])
                    if oc % 2 == 0:
                        P.op("vector", lambda e, oc=oc, p_=p_: e.tensor_copy(out=qmT[:, oc, :], in_=p_[:, :]), reads=[pB_], writes=[qmTB])
                    else:
                        P.op("scalar", lambda e, oc=oc, p_=p_: e.copy(out=qmT[:, oc, :], in_=p_[:, :]), reads=[pB_], writes=[qmTB])
                for hd in range(4):
                    for mt in range(2):
                        P.group("tensor", [(lambda e, cc=cc, hd=hd, mt=mt: e.matmul(
                            pS[mt][:, :], lhsT=KmT[:, 2 * hd + cc, mt * 128:(mt + 1) * 128], rhs=qmT[:, 2 * hd + cc, :],
                            start=(cc == 0), stop=(cc == 1))) for cc in range(2)], reads=[KMT, qmTB], writes=[pSB[mt]])
                        P.op("scalar", lambda e, mt=mt: e.activation(out=Et[mt][:], in_=pS[mt][:, :], func=AF.Exp, scale=1.0 / 16.0),
                             reads=[pSB[mt]], writes=[EtB[mt]])
                    P.group("tensor", [(lambda e, mt=mt: e.matmul(pSum[:, :], lhsT=ones_bf[:], rhs=Et[mt][:],
                                                                   start=(mt == 0), stop=(mt == 1))) for mt in range(2)],
                            reads=[EtB[0], EtB[1], CC, CD], writes=[pSumB])
                    P.op("vector", lambda e: e.reciprocal(out=rinv[:], in_=pSum[:, :]), reads=[pSumB], writes=[rinvB])
                    for dc in range(2):
                        P.group("tensor", [(lambda e, mt=mt, hd=hd, dc=dc: e.matmul(
                            pO[:, :], lhsT=Vm[:, mt, hd * 256 + dc * 128:hd * 256 + (dc + 1) * 128], rhs=Et[mt][:],
                            start=(mt == 0), stop=(mt == 1))) for mt in range(2)], reads=[VM, EtB[0], EtB[1]], writes=[pOB])
                        P.op("vector", lambda e, hd=hd, dc=dc: e.tensor_tensor(out=OT[:, hd * 2 + dc, :], in0=pO[:, :], in1=rinv[:],
                                                                               op=ALU.mult), reads=[pOB, rinvB], writes=[OTB])
                for j in range(4):
                    ti = st * 4 + j
                    for hf in range(2):
                        p_, pB_ = next_pmm()
                        P.group("tensor", [(lambda e, kc=kc, j=j, hf=hf, p_=p_: e.matmul(
                            p_[:, :], lhsT=OT[:, kc, j * 128:(j + 1) * 128], rhs=wo_bf[:, kc, hf * 512:(hf + 1) * 512],
                            start=(kc == 0), stop=(kc == 7))) for kc in range(8)], reads=[WO, OTB], writes=[pB_])
                        P.op("vector", lambda e, j=j, hf=hf, p_=p_: e.scalar_tensor_tensor(
                            out=pre[:, hf * 512:(hf + 1) * 512], in0=h1r[:, j, hf * 512:(hf + 1) * 512], scalar=ALPHA,
                            in1=p_[:, :], op0=ALU.mult, op1=ALU.add), reads=[h1rB[j], pB_], writes=[preB])
                    b2 = ti % 2
                    layer_norm_tile(pre[:], h2t[b2][:], g_2, bb_2, preB, h2tB[b2], lnt, LNT)
                    P.dma("gpsimd", lambda e, ti=ti, b2=b2: e.dma_start(out=h2s[ti * 128:(ti + 1) * 128, :], in_=h2t[b2][:]),
                          H2S[b2], reads=[h2tB[b2]], stream=True)
                    P.op("scalar", lambda e, b2=b2: e.copy(out=h2b[b2][:], in_=h2t[b2][:]), reads=[h2tB[b2]], writes=[h2bB[b2]])
                    for g4 in range(2):
                        pq, pqB = next_pmm()
                        P.group("tensor", [(lambda e, c=c, g4=g4, pq=pq, b2=b2: e.transpose(
                            out=pq[:, c * 128:(c + 1) * 128], in_=h2t[b2][:, (g4 * 4 + c) * 128:(g4 * 4 + c + 1) * 128],
                            identity=ident_f[:])) for c in range(4)], reads=[h2tB[b2], CC, CD], writes=[pqB])
                        P.op("vector", lambda e, g4=g4, pq=pq: e.tensor_copy(
                            out=h2T[:, g4 * 4:(g4 + 1) * 4, :], in_=pq[:, :].rearrange("p (c t) -> p c t", c=4)),
                            reads=[pqB], writes=[h2TB])
                    pl, plB = next_pmm()
                    fns = [(lambda e, kc=kc, pl=pl: e.matmul(pl[:, 0:NE], lhsT=h2T[:, kc, :], rhs=wr_f[:, kc, :],
                                                             start=(kc == 0), stop=False)) for kc in range(8)]
                    fns.append(lambda e, pl=pl: e.matmul(pl[:, 0:NE], lhsT=ones_f[0:1, :], rhs=brr_f[0:1, :], start=False, stop=True))
                    P.group("tensor", fns, reads=[h2TB, CC, CD], writes=[plB])
                    P.op("vector", lambda e, pl=pl: e.tensor_copy(out=lg[:], in_=pl[:, 0:NE]), reads=[plB], writes=[lgB])
                    P.op("vector", lambda e: e.max(out=m8[:], in_=lg[:]), reads=[lgB], writes=[m8B])
                    P.op("vector", lambda e: e.tensor_scalar(out=msk[:], in0=lg[:], scalar1=m8[:, 3:4], scalar2=None, op0=ALU.is_ge),
                         reads=[lgB, m8B], writes=[mskB])
                    P.op("vector", lambda e: e.tensor_scalar(out=nm[:], in0=m8[:, 0:1], scalar1=-1.0, scalar2=None, op0=ALU.mult),
                         reads=[m8B], writes=[exB])
                    P.op("scalar", lambda e: e.activation(out=ex[:], in_=lg[:], func=AF.Exp, bias=nm[:, 0:1]),
                         reads=[lgB, exB], writes=[exB])
                    P.op("vector", lambda e: e.tensor_tensor(out=ex[:], in0=ex[:], in1=msk[:], op=ALU.mult), reads=[exB, mskB], writes=[exB])
                    P.op("vector", lambda e: e.reduce_sum(out=ssum[:, 0:1], in_=ex[:], axis=AX.X), reads=[exB], writes=[GB])
                    P.op("vector", lambda e: e.reciprocal(out=ssum[:, 1:2], in_=ssum[:, 0:1]), reads=[GB], writes=[GB])
                    P.op("vector", lambda e: e.tensor_scalar(out=G[:], in0=ex[:], scalar1=ssum[:, 1:2], scalar2=None, op0=ALU.mult),
                         reads=[exB, GB], writes=[GB])
                    P.op("vector", lambda e: e.tensor_copy(out=mskb[:], in_=msk[:]), reads=[mskB], writes=[mskbB])
                    pp, ppB = next_pmm()
                    P.group("tensor", [lambda e, pp=pp: e.matmul(pp[:, 0:NE], lhsT=maskD[:, 0:128], rhs=mskb[:], start=True, stop=False),
                                       lambda e, pp=pp: e.matmul(pp[:, 0:NE], lhsT=ones_bf[:], rhs=cumb[:], start=False, stop=True)],
                            reads=[mskbB, cumbB, CC, CD], writes=[ppB])
                    P.op("vector", lambda e, pp=pp: e.tensor_tensor(out=posf[:], in0=pp[:, 0:NE], in1=eoff[:], op=ALU.add),
                         reads=[ppB, CC, CD], writes=[posB])
                    P.op("vector", lambda e: e.tensor_tensor(out=cum[:], in0=cum[:], in1=msk[:], op=ALU.add), reads=[cumB, mskB], writes=[cumB])
                    P.op("vector", lambda e: e.tensor_copy(out=cumb[:], in_=cum[:]), reads=[cumB], writes=[cumbB])
                    for k in range(4):
                        col = ti * 4 + k
                        P.op("vector", lambda e, k=k: e.tensor_scalar(out=oh[:], in0=lg[:], scalar1=m8[:, k:k + 1], scalar2=None,
                                                                       op0=ALU.is_equal), reads=[lgB, m8B], writes=[ohB])
                        P.op("vector", lambda e: e.tensor_tensor(out=tmp32[:], in0=oh[:], in1=posf[:], op=ALU.mult),
                             reads=[ohB, posB], writes=[t32B])
                        P.op("vector", lambda e, k=k: e.reduce_sum(out=dstf[:, k:k + 1], in_=tmp32[:], axis=AX.X),
                             reads=[t32B], writes=[dstfB])
                        P.op("vector", lambda e: e.tensor_tensor(out=tmp32[:], in0=oh[:], in1=G[:], op=ALU.mult),
                             reads=[ohB, GB], writes=[t32B])
                        P.op("vector", lambda e, col=col: e.reduce_sum(out=gates_all[:, col:col + 1], in_=tmp32[:], axis=AX.X),
                             reads=[t32B], writes=[GD])
                    P.op("vector", lambda e, ti=ti: e.tensor_copy(out=dest_all[:, ti * 4:ti * 4 + 4], in_=dstf[:]),
                         reads=[dstfB], writes=[GD])
                    for k in range(4):
                        col = ti * 4 + k
                        P.dma("gpsimd", lambda e, col=col, b2=b2: e.indirect_dma_start(
                            out=Xe[:, :], out_offset=IndirectOffsetOnAxis(ap=dest_all[:, col:col + 1], axis=0),
                            in_=h2b[b2][:], in_offset=None), XE[b2], reads=[h2bB[b2], GD], stream=True)
            P.barrier()
            P.flush()

        with ExitStack() as es:
          if upto >= 3:
            bgu = sb(es, "bgu", [128, NE, 16], F32)
            P.dma("sync", lambda e: e.dma_start(out=bgu[:], in_=b_gu_p), CD, stream=True)
            wgu_bf = [sb(es, "wgu%d" % i, [128, 8, 2048], BF16) for i in range(2)]; WGU = [P.buf("wgu%d" % i) for i in range(2)]
            wdn_bf = [sb(es, "wdn%d" % i, [128, 8, D], BF16) for i in range(2)]; WDN = [P.buf("wdn%d" % i) for i in range(2)]
            bdn_b = [sb(es, "bdnb%d" % i, [1, D], BF16) for i in range(2)]; BDNB = [P.buf("bdnb%d" % i) for i in range(2)]
            pT = [ps(es, "pT%d" % i, [128, 1024], BF16) for i in range(2)]
            pTB = [P.buf() for _ in range(2)]
            pg = [ps(es, "pg%d" % i, [128, 512], F32) for i in range(2)]; pgB = [P.buf() for _ in range(2)]
            pu = [ps(es, "pu%d" % i, [128, 512], F32) for i in range(2)]; puB = [P.buf() for _ in range(2)]
            py = [ps(es, "py%d" % i, [128, 512], F32) for i in range(2)]; pyB = [P.buf() for _ in range(2)]
            xe_t = [sb(es, "xe%d" % i, [128, D], BF16) for i in range(2)]; xeB = [P.buf("xe%d" % i) for i in range(2)]
            xT = [sb(es, "xT%d" % i, [128, 8, CAP], BF16) for i in range(2)]; xTB = [P.buf() for _ in range(2)]
            actT = sb(es, "actT", [128, 8, CAP], BF16); actB = P.buf()
            gc = [sb(es, "gc%d" % i, [128, HALF], F32) for i in range(2)]; gcB = [P.buf() for _ in range(2)]
            sgm = [sb(es, "sgm%d" % i, [128, HALF], F32) for i in range(2)]; sgmB = [P.buf() for _ in range(2)]
            u1 = [sb(es, "u1%d" % i, [128, HALF], F32) for i in range(2)]; u1B = [P.buf() for _ in range(2)]
            yt = [sb(es, "yt%d" % i, [128, D], F32) for i in range(2)]; ytB = [P.buf("yt%d" % i) for i in range(2)]
            tpi = [0]; xei = [0]; cnt2 = [0]; yi = [0]; pyi = [0]

            def load_expert(e_):
                b = e_ % 2
                for c in range(8):
                    load_cast(w_gu[e_, c * 128:(c + 1) * 128, :], wgu_bf[b][:, c, :], WGU[b], 2048, eng="gpsimd")
                wv = w_dn[e_].rearrange("(c p) n -> p c n", p=128)
                for c in range(0, 8, 2):
                    load_cast(wv[:, c:c + 2, :], wdn_bf[b][:, c:c + 2, :], WDN[b], 2048, eng="gpsimd")
                P.dma("gpsimd", lambda e: e.dma_start(out=bdn_b[b][:], in_=b_dn[e_:e_ + 1, :]), BDNB[b], stream=True, war=True)

            load_expert(0)
            for e_ in range(NE):
                b = e_ % 2
                if e_ + 1 < NE:
                    load_expert(e_ + 1)
                for stl in range(CAP // 128):
                    xi = xei[0] % 2
                    xei[0] += 1
                    r0 = e_ * CAP + stl * 128
                    P.dma("sync", lambda e, xi=xi, r0=r0: e.dma_start(out=xe_t[xi][:], in_=Xe[r0:r0 + 128, :]), xeB[xi], reads=XE)
                    i = tpi[0] % 2
                    tpi[0] += 1
                    transpose_to(xe_t[xi], xeB[xi], xT[b][:, :, stl * 128:(stl + 1) * 128], xTB[b], pT[i], pTB[i], eng="scalar")
                for hf in range(2):
                    s0 = hf * HALF
                    for jc in range(8):
                        k2 = cnt2[0] % 2
                        cnt2[0] += 1
                        P.group("tensor", [(lambda e, kc=kc, jc=jc, k2=k2, s0=s0, b=b: e.matmul(
                            pg[k2][:, 0:HALF], lhsT=wgu_bf[b][:, kc, jc * 128:(jc + 1) * 128], rhs=xT[b][:, kc, s0:s0 + HALF],
                            start=(kc == 0), stop=(kc == 7))) for kc in range(8)], reads=[WGU[b], xTB[b]], writes=[pgB[k2]])
                        P.group("tensor", [(lambda e, kc=kc, jc=jc, k2=k2, s0=s0, b=b: e.matmul(
                            pu[k2][:, 0:HALF], lhsT=wgu_bf[b][:, kc, 1024 + jc * 128:1024 + (jc + 1) * 128],
                            rhs=xT[b][:, kc, s0:s0 + HALF], start=(kc == 0), stop=(kc == 7))) for kc in range(8)],
                            reads=[WGU[b], xTB[b]], writes=[puB[k2]])
                        P.op("vector", lambda e, k2=k2, jc=jc, e_=e_: e.tensor_scalar(
                            out=gc[k2][:], in0=pg[k2][:, 0:HALF], scalar1=bgu[:, e_, jc:jc + 1], scalar2=7.0,
                            op0=ALU.add, op1=ALU.min), reads=[pgB[k2], CC, CD], writes=[gcB[k2]])
                        P.op("scalar", lambda e, k2=k2: e.activation(out=sgm[k2][:], in_=gc[k2][:], func=AF.Sigmoid, scale=1.702),
                             reads=[gcB[k2]], writes=[sgmB[k2]])
                        P.op("vector", lambda e, k2=k2, jc=jc, e_=e_: e.tensor_scalar(
                            out=u1[k2][:], in0=pu[k2][:, 0:HALF], scalar1=bgu[:, e_, 8 + jc:9 + jc], scalar2=-7.0,
                            op0=ALU.add, op1=ALU.max), reads=[puB[k2], CC, CD], writes=[u1B[k2]])
                        P.op("vector", lambda e, k2=k2: e.tensor_tensor(out=gc[k2][:], in0=gc[k2][:], in1=sgm[k2][:], op=ALU.mult),
                             reads=[gcB[k2], sgmB[k2]], writes=[gcB[k2]])
                        P.op("vector", lambda e, k2=k2: e.tensor_scalar(out=u1[k2][:], in0=u1[k2][:], scalar1=7.0, scalar2=1.0,
                                                                         op0=ALU.min, op1=ALU.add), reads=[u1B[k2]], writes=[u1B[k2]])
                        P.op("vector", lambda e, k2=k2, jc=jc, s0=s0: e.tensor_tensor(
                            out=actT[:, jc, s0:s0 + HALF], in0=u1[k2][:], in1=gc[k2][:], op=ALU.mult),
                            reads=[u1B[k2], gcB[k2]], writes=[actB])
                for stl in range(CAP // 128):
                    y2 = yi[0] % 2
                    yi[0] += 1
                    for hf in range(2):
                        p2 = pyi[0] % 2
                        pyi[0] += 1
                        fns = [(lambda e, jc=jc, stl=stl, hf=hf, p2=p2, b=b: e.matmul(
                            py[p2][:, :], lhsT=actT[:, jc, stl * 128:(stl + 1) * 128], rhs=wdn_bf[b][:, jc, hf * 512:(hf + 1) * 512],
                            start=(jc == 0), stop=False)) for jc in range(8)]
                        fns.append(lambda e, hf=hf, p2=p2, b=b: e.matmul(py[p2][:, :], lhsT=ones_bf[0:1, :],
                                                                         rhs=bdn_b[b][0:1, hf * 512:(hf + 1) * 512], start=False, stop=True))
                        P.group("tensor", fns, reads=[actB, WDN[b], BDNB[b], CC, CD], writes=[pyB[p2]])
                        P.op("scalar", lambda e, hf=hf, p2=p2, y2=y2: e.copy(out=yt[y2][:, hf * 512:(hf + 1) * 512], in_=py[p2][:, :]),
                             reads=[pyB[p2]], writes=[ytB[y2]])
                    r0 = e_ * CAP + stl * 128
                    P.dma("gpsimd", lambda e, r0=r0, y2=y2: e.dma_start(out=Ys[r0:r0 + 128, :], in_=yt[y2][:]), YS[y2],
                          reads=[ytB[y2]], stream=True)
            P.barrier()
            P.flush()

        with ExitStack() as es:
          if upto >= 4:
            g_3 = bc_load(es, "g_3", ln3_g); bb_3 = bc_load(es, "bb_3", ln3_b)
            st_t = sb(es, "st_t", [128, 12], F32); mv_t = sb(es, "mv_t", [128, 2], F32); rs_t = sb(es, "rs_t", [128, 2], F32)
            LNT = P.buf("lntF")
            lnt = (st_t, mv_t, rs_t)
            h2r = [sb(es, "h2r%d" % i, [128, D], F32) for i in range(2)]; h2rB = [P.buf("h2r%d" % i) for i in range(2)]
            yk = [[sb(es, "yk%d_%d" % (i, k), [128, D], F32) for k in range(4)] for i in range(2)]
            ykB = [[P.buf("yk%d_%d" % (i, k)) for k in range(4)] for i in range(2)]
            accF = sb(es, "accF", [128, D], F32); accFB = P.buf()
            ot = [sb(es, "ot%d" % i, [128, D], F32) for i in range(2)]; otB = [P.buf("ot%d" % i) for i in range(2)]
            for ti in range(NT):
                b = ti % 2
                P.dma("sync", lambda e, ti=ti, b=b: e.dma_start(out=h2r[b][:], in_=h2s[ti * 128:(ti + 1) * 128, :]), h2rB[b], reads=H2S)
                for k in range(4):
                    col = ti * 4 + k
                    P.dma("gpsimd", lambda e, col=col, b=b, k=k: e.indirect_dma_start(
                        out=yk[b][k][:], out_offset=None, in_=Ys[:, :],
                        in_offset=IndirectOffsetOnAxis(ap=dest_all[:, col:col + 1], axis=0)), ykB[b][k], reads=YS + [GD])
                P.op("scalar", lambda e, b=b: e.mul(out=accF[:], in_=h2r[b][:], mul=ALPHA), reads=[h2rB[b]], writes=[accFB])
                for k in range(4):
                    col = ti * 4 + k
                    P.op("vector", lambda e, b=b, k=k, col=col: e.scalar_tensor_tensor(
                        out=accF[:], in0=yk[b][k][:], scalar=gates_all[:, col:col + 1], in1=accF[:], op0=ALU.mult, op1=ALU.add),
                        reads=[ykB[b][k], GD, accFB], writes=[accFB])
                layer_norm_tile(accF[:], ot[b][:], g_3, bb_3, accFB, otB[b], lnt, LNT)
                P.dma("sync", lambda e, ti=ti, b=b: e.dma_start(out=out[ti * 128:(ti + 1) * 128, :], in_=ot[b][:]), OUTB[b],
                      reads=[otB[b]], stream=True)
            P.barrier()
            P.flush()
    return nc


_NC_CACHE = {}


def kernel(**inputs):
    f = lambda a: np.ascontiguousarray(np.asarray(a, dtype=np.float32))
    x = f(inputs["x"]); mem = f(inputs["mem"])
    B = x.shape[0]
    b_in = f(inputs["b_in"])[0]
    w_dw = f(inputs["w_dw"])[0]
    shared = {
        "ln_in_g": f(inputs["ln_in_g"]), "ln_in_b": f(inputs["ln_in_b"]),
        "ln_mem_g": f(inputs["ln_mem_g"]), "ln_mem_b": f(inputs["ln_mem_b"]),
        "w_in": f(inputs["w_in"])[0],
        "b_in_p": np.ascontiguousarray(b_in.reshape(20, 128).T),
        "b_v": np.ascontiguousarray(b_in[2048:2560].reshape(1, 512)),
        "w_dw_p": np.ascontiguousarray(w_dw.T.reshape(4, 128, 31).transpose(1, 0, 2)),
        "b_dw_p": np.ascontiguousarray(f(inputs["b_dw"])[0].reshape(4, 128).T),
        "lncg_p": np.ascontiguousarray(f(inputs["ln_conv_g"])[0].reshape(4, 128).T),
        "lncb_p": np.ascontiguousarray(f(inputs["ln_conv_b"])[0].reshape(4, 128).T),
        "w_out": f(inputs["w_out"])[0], "b_out": f(inputs["b_out"])[0].reshape(1, D),
        "ln1_g": f(inputs["ln1_g"])[0], "ln1_b": f(inputs["ln1_b"])[0],
        "w_q": f(inputs["w_q_mem"])[0], "w_k": f(inputs["w_k_mem"])[0],
        "w_v": f(inputs["w_v_mem"])[0], "w_o": f(inputs["w_o_mem"])[0],
        "ln2_g": f(inputs["ln2_g"])[0], "ln2_b": f(inputs["ln2_b"])[0],
        "w_r": f(inputs["w_router"])[0], "b_r": f(inputs["b_router"])[0].reshape(1, NE),
        "w_gu": f(inputs["w_gu"])[0],
        "b_gu_p": np.ascontiguousarray(f(inputs["b_gu"])[0].reshape(NE, 16, 128).transpose(2, 0, 1)),
        "w_dn": f(inputs["w_down"])[0], "b_dn": f(inputs["b_down"])[0],
        "ln3_g": f(inputs["ln3_g"])[0], "ln3_b": f(inputs["ln3_b"])[0],
    }
    if "nc" not in _NC_CACHE:
        _NC_CACHE["nc"] = build_program()
    nc = _NC_CACHE["nc"]
    in_maps = []
    for b in range(B):
        m = dict(shared)
        m["x"] = np.ascontiguousarray(x[b])
        m["mem"] = np.ascontiguousarray(mem[b])
        in_maps.append(m)
    res = run_bass_kernel_spmd(nc, in_maps, core_ids=list(range(B)))
    return np.stack([np.asarray(r["out"], dtype=np.float32) for r in res.results], axis=0)
```

```python
import numpy as np
from contextlib import ExitStack
import concourse.bass as bass
import concourse.mybir as mybir
from concourse.bass_utils import run_bass_kernel_spmd
from concourse.bass import IndirectOffsetOnAxis

F32 = mybir.dt.float32
BF16 = mybir.dt.bfloat16
I32 = mybir.dt.int32
AF = mybir.ActivationFunctionType
ALU = mybir.AluOpType
AX = mybir.AxisListType
ENGS = ["tensor", "vector", "scalar", "gpsimd", "sync"]

T = 4096
D = 1024
NM = 256
NE = 32
CAP = 1536
NSLOT = NE * CAP
ALPHA = 2.0 ** 0.25
EPS = 1e-5
NT = T // 128
NST = T // 512
PADL = 30
BLK = 256
NBLK = CAP // BLK


class Buf:
    def __init__(self, name):
        self.name = name
        self.w = None
        self.r = {}
        self.sem = None


class Prog:
    def __init__(self, nc, es):
        self.nc = nc
        self.es = es
        self.q = {e: [] for e in ENGS}
        self.cnt = {}
        self.known = {e: {} for e in ENGS}
        self.sems = {}
        self.nbuf = 0
        self.cond = None
        self.regs = {}
        self.cnt_ap = None
        for e in ENGS:
            self.newsem("E_" + e)

    def begin_cond(self, tag):
        self.cond = tag
        self._saved_known = {e: dict(k) for e, k in self.known.items()}

    def end_cond(self):
        self.cond = None
        self.known = self._saved_known

    def regload(self, idx, slot):
        for e in ENGS:
            self.q[e].append(("regload", idx, slot))

    def newsem(self, name):
        self.sems[name] = self.es.enter_context(self.nc.semaphore(name))
        self.cnt[name] = 0
        return name

    def buf(self, name=None):
        self.nbuf += 1
        return Buf(name or ("b%d" % self.nbuf))

    def _waits(self, eng, deps):
        for d in deps:
            if d is None:
                continue
            s, v = d
            if self.known[eng].get(s, 0) >= v:
                continue
            self.known[eng][s] = v
            self.q[eng].append(("wait", s, v, self.cond))

    def _deps(self, eng, reads, writes):
        own = "E_" + eng
        deps = []
        for b in reads:
            if b.w is not None:
                deps.append(b.w)
        for b in writes:
            if b.w is not None and b.w[0] != own:
                deps.append(b.w)
            for s, v in b.r.items():
                if s != own:
                    deps.append((s, v))
        return deps

    def _mark(self, tok, reads, writes):
        for b in reads:
            b.r[tok[0]] = max(b.r.get(tok[0], 0), tok[1])
        for b in writes:
            b.w = tok
            b.r = {}

    def op(self, eng, fn, reads=(), writes=()):
        self._waits(eng, self._deps(eng, reads, writes))
        s = "E_" + eng
        self.cnt[s] += 1
        self.q[eng].append(("op", fn, s, 1, self.cond, self.cnt[s]))
        tok = (s, self.cnt[s])
        self._mark(tok, reads, writes)
        return tok

    def group(self, eng, fns, reads=(), writes=()):
        self._waits(eng, self._deps(eng, reads, writes))
        s = "E_" + eng
        for fn in fns:
            self.cnt[s] += 1
            self.q[eng].append(("op", fn, s, 1, self.cond, self.cnt[s]))
        tok = (s, self.cnt[s])
        self._mark(tok, reads, writes)
        return tok

    def dma(self, eng, fn, dst, reads=(), stream=False, war=False):
        if dst.sem is None:
            dst.sem = self.newsem("D_" + dst.name)
        deps = self._deps(eng, reads, () if stream else (dst,))
        if stream and war:
            deps = deps + [(s_, v_) for s_, v_ in dst.r.items()]
        self._waits(eng, deps)
        self.cnt[dst.sem] += 16
        self.q[eng].append(("op", fn, dst.sem, 16, self.cond, self.cnt[dst.sem]))
        tok = (dst.sem, self.cnt[dst.sem])
        for b in reads:
            b.r[tok[0]] = max(b.r.get(tok[0], 0), tok[1])
        if stream:
            dst.w = tok
        else:
            dst.w = tok
            dst.r = {}
        return tok

    def barrier(self):
        for e in ENGS:
            self._waits(e, [(s, v) for s, v in self.cnt.items() if v > 0])

    def flush(self):
        nc = self.nc
        q = self.q
        self.q = {e: [] for e in ENGS}
        sems = self.sems
        with nc.Block() as block:
            def emit1(e, it):
                if it[0] == "wait":
                    e.wait_ge(sems[it[1]], it[2])
                else:
                    it[1](e).then_inc(sems[it[2]], it[3])

            def tagof(it):
                return it[3] if it[0] == "wait" else it[4]

            def replay(e, name):
                items = q[name]
                i = 0
                while i < len(items):
                    it = items[i]
                    if it[0] == "regload":
                        if name not in self.regs:
                            self.regs[name] = [e.alloc_register("cntreg%d_%s" % (k_, name)) for k_ in range(2)]
                        e.reg_load(self.regs[name][it[2]], self.cnt_ap[0:1, it[1]:it[1] + 1])
                        i += 1
                        continue
                    tag = tagof(it)
                    if tag is None:
                        emit1(e, it)
                        i += 1
                        continue
                    j = i
                    while j < len(items) and items[j][0] != "regload" and tagof(items[j]) == tag:
                        j += 1
                    grp = items[i:j]
                    i = j
                    incs = {}
                    for it2 in grp:
                        if it2[0] == "op":
                            s_ = it2[2]
                            if s_ not in incs:
                                incs[s_] = [it2[5] - it2[3], 0]
                            incs[s_][1] += it2[3]
                    if not incs:
                        continue
                    with e.If_cmp(self.regs[name][tag[2]], tag[1], "IS_GT"):
                        for it2 in grp:
                            emit1(e, it2)
                    with e.Else():
                        for s_, (before, total) in incs.items():
                            if before > 0:
                                e.wait_ge(sems[s_], before)
                            e.sem_inc(sems[s_], total)

            @block.sync
            def _(e):
                replay(e, "sync")

            @block.tensor
            def _(e):
                replay(e, "tensor")

            @block.vector
            def _(e):
                replay(e, "vector")

            @block.scalar
            def _(e):
                replay(e, "scalar")

            @block.gpsimd
            def _(e):
                replay(e, "gpsimd")


def build_program(dbg=False, upto=4, sub=99, nst_run=NST, sub2=99):
    nc = bass.Bass("TRN2", target_bir_lowering=False)

    def din(name, shape, dt=F32):
        return nc.dram_tensor(name, list(shape), dt, kind="ExternalInput").ap()

    def dscr(name, shape, dt):
        return nc.dram_tensor(name, list(shape), dt, kind=("ExternalOutput" if dbg else "Internal")).ap()

    x = din("x", [T, D])
    mem = din("mem", [NM, D])
    ln_in_g = din("ln_in_g", [D]); ln_in_b = din("ln_in_b", [D])
    ln_mem_g = din("ln_mem_g", [D]); ln_mem_b = din("ln_mem_b", [D])
    w_in = din("w_in", [D, 2560]); b_in_p = din("b_in_p", [128, 20]); b_v = din("b_v", [1, 512])
    w_dw_p = din("w_dw_p", [128, 4, 31]); b_dw_p = din("b_dw_p", [128, 4])
    lncg_p = din("lncg_p", [128, 4]); lncb_p = din("lncb_p", [128, 4])
    w_out = din("w_out", [D, D]); b_out = din("b_out", [1, D])
    ln1_g = din("ln1_g", [D]); ln1_b = din("ln1_b", [D])
    w_q = din("w_q", [D, D]); w_k = din("w_k", [D, D]); w_v = din("w_v", [D, D]); w_o = din("w_o", [D, D])
    ln2_g = din("ln2_g", [D]); ln2_b = din("ln2_b", [D])
    w_r = din("w_r", [D, NE]); b_r = din("b_r", [1, NE])
    w_gu = din("w_gu", [NE, D, 2048]); b_gu_p = din("b_gu_p", [128, NE, 16])
    w_dn = din("w_dn", [NE, D, D]); b_dn = din("b_dn", [NE, D])
    ln3_g = din("ln3_g", [D]); ln3_b = din("ln3_b", [D])
    out = nc.dram_tensor("out", [T, D], F32, kind="ExternalOutput").ap()

    h1s = dscr("h1s", [T, D], F32)
    h2s = dscr("h2s", [T, D], F32)
    Xe = nc.dram_tensor("Xe", [NSLOT, D], BF16, kind="Internal").ap()
    Ys = nc.dram_tensor("Ys", [NSLOT, D], F32, kind="Internal").ap()
    cntd = nc.dram_tensor("cntd", [1, NE], I32, kind="Internal").ap()
    if dbg:
        catd = nc.dram_tensor("catd", [NST, 128, 8, 512], BF16, kind="ExternalOutput").ap()
        hd = nc.dram_tensor("hd", [NST, 128, 4, D], F32, kind="ExternalOutput").ap()

    with ExitStack() as es0:
        P = Prog(nc, es0)

        uid = [0]

        def sb(es, name, shape, dt):
            uid[0] += 1
            return es.enter_context(nc.sbuf_tensor("%s_%d" % (name, uid[0]), list(shape), dt))

        def ps(es, name, shape, dt):
            uid[0] += 1
            return es.enter_context(nc.psum_tensor("%s_%d" % (name, uid[0]), list(shape), dt))

        ident = sb(es0, "ident", [128, 128], BF16)
        ident_f = sb(es0, "ident_f", [128, 128], F32)
        ones_bf = sb(es0, "ones_bf", [128, 128], BF16)
        ones_f = sb(es0, "ones_f", [128, 128], F32)
        onesS = sb(es0, "onesS", [128, 128], F32)
        triInc = sb(es0, "triInc", [128, 128], BF16)
        maskD = sb(es0, "maskD", [128, 512], BF16)
        gates_all = sb(es0, "gates_all", [128, NT * 4], F32)
        dest_all = sb(es0, "dest_all", [128, NT * 4], I32)
        KmT = sb(es0, "KmT", [128, 8, NM], BF16)
        Vm = sb(es0, "Vm", [128, 2, D], BF16)
        CC = P.buf("constc"); CD = P.buf("constd")
        GD = P.buf("gd")
        DBGB = P.buf("dbg")

        def load_cast(src_ap, dst_ap, dstbuf, width, eng=None):
            P.dma("gpsimd", lambda e: e.dma_start(out=dst_ap, in_=src_ap), dstbuf, stream=True, war=True)

        def load_w(es, name, w_ap, kdim, ndim, wbuf):
            t = sb(es, name, [128, kdim // 128, ndim], BF16)
            wv = w_ap.rearrange("(c p) n -> p c n", p=128)
            per = min(kdim // 128, max(1, 2048 // ndim))
            if ndim > 2048:
                for c in range(kdim // 128):
                    for n0 in range(0, ndim, 1280):
                        load_cast(wv[:, c, n0:n0 + 1280], t[:, c, n0:n0 + 1280], wbuf, 1280)
            else:
                for c in range(0, kdim // 128, per):
                    load_cast(wv[:, c:c + per, :], t[:, c:c + per, :], wbuf, per * ndim)
            return t

        def bc_load(es, name, vec_ap, n=D):
            t = sb(es, name, [128, n], F32)
            P.dma("sync", lambda e: e.dma_start(out=t[:], in_=vec_ap.partition_broadcast(128)), CD, stream=True)
            return t

        def layer_norm_tile(*a, **k):
            for _ in ln_gen(*a, **k):
                pass

        def ln_gen(src, dst, g_t, b_t, srcB, dstB, tmpst, tmpB, n=D):
            st_t, mv_t, rs_t = tmpst
            nch = n // 512
            srcBs = list(srcB) if isinstance(srcB, (list, tuple)) else [srcB]
            yield P.group("vector", [(lambda e, c=c: e.bn_stats(out=st_t[:, c * 6:(c + 1) * 6], in_=src[:, c * 512:(c + 1) * 512]))
                               for c in range(nch)], reads=srcBs, writes=[tmpB])
            yield P.op("vector", lambda e: e.bn_aggr(out=mv_t[:], in_=st_t[:, 0:nch * 6]), reads=[tmpB], writes=[tmpB])
            yield P.op("scalar", lambda e: e.activation(out=rs_t[:, 0:1], in_=mv_t[:, 1:2], func=AF.Ln, bias=EPS_T[:, 0:1]),
                 reads=[tmpB, CC, CD], writes=[tmpB])
            yield P.op("scalar", lambda e: e.activation(out=rs_t[:, 1:2], in_=rs_t[:, 0:1], func=AF.Exp, scale=-0.5),
                 reads=[tmpB], writes=[tmpB])
            yield P.op("vector", lambda e: e.tensor_scalar(out=dst, in0=src, scalar1=mv_t[:, 0:1], scalar2=rs_t[:, 1:2],
                                                     op0=ALU.subtract, op1=ALU.mult),
                 reads=srcBs + [tmpB], writes=[dstB])
            yield P.op("gpsimd", lambda e: e.tensor_tensor(out=dst, in0=dst, in1=g_t[:, 0:n], op=ALU.mult),
                 reads=[dstB, CC, CD], writes=[dstB])
            yield P.op("gpsimd", lambda e: e.tensor_tensor(out=dst, in0=dst, in1=b_t[:, 0:n], op=ALU.add),
                 reads=[dstB, CC, CD], writes=[dstB])

        EPS_T = sb(es0, "eps_t", [128, 1], F32)
        P.op("gpsimd", lambda e: e.memset(EPS_T[:], EPS), writes=[CC])
        ONE_T = sb(es0, "one_t", [128, 1], F32)
        P.op("gpsimd", lambda e: e.memset(ONE_T[:], 1.0), writes=[CC])

        def transpose_to(src_bf, srcB, dstT_ap, dstB, pT, pTB, nchunks=8, eng="scalar"):
            P.group("tensor", [(lambda e, c=c: e.transpose(out=pT[:, c * 128:(c + 1) * 128],
                                                           in_=src_bf[:, c * 128:(c + 1) * 128], identity=ident[:]))
                               for c in range(nchunks)], reads=[srcB, CC, CD], writes=[pTB])
            pv = pT[:, 0:nchunks * 128].rearrange("p (c t) -> p c t", c=nchunks)
            if eng == "scalar":
                P.op("scalar", lambda e: e.copy(out=dstT_ap, in_=pv), reads=[pTB], writes=[dstB])
            else:
                P.op(eng, lambda e: e.tensor_copy(out=dstT_ap, in_=pv), reads=[pTB], writes=[dstB])

        with ExitStack() as est:
            tmpf = sb(est, "tmpf", [128, 512], F32)
            P.op("gpsimd", lambda e: e.memset(ones_f[:], 1.0), writes=[CC])
            P.op("gpsimd", lambda e: e.memset(onesS[:], 1.0 / 512.0), writes=[CC])
            P.op("gpsimd", lambda e: e.memset(tmpf[:], 1.0), writes=[CC])
            P.op("vector", lambda e: e.tensor_copy(out=ones_bf[:], in_=ones_f[:]), reads=[CC, CD], writes=[CC])
            tA = sb(est, "tA", [128, 128], F32)
            P.op("gpsimd", lambda e: e.affine_select(out=tA[:], in_=ones_f[:], pattern=[[1, 128]],
                                                     compare_op=ALU.is_equal, fill=0.0, base=0,
                                                     channel_multiplier=-1), reads=[CC, CD], writes=[CC])
            P.op("vector", lambda e: e.tensor_copy(out=ident[:], in_=tA[:]), reads=[CC, CD], writes=[CC])
            P.op("vector", lambda e: e.tensor_copy(out=ident_f[:], in_=tA[:]), reads=[CC, CD], writes=[CC])
            tB = sb(est, "tB", [128, 128], F32)
            P.op("gpsimd", lambda e: e.affine_select(out=tB[:], in_=ones_f[:], pattern=[[-1, 128]],
                                                     compare_op=ALU.is_ge, fill=0.0, base=0,
                                                     channel_multiplier=1), reads=[CC, CD], writes=[CC])
            P.op("vector", lambda e: e.tensor_copy(out=triInc[:], in_=tB[:]), reads=[CC, CD], writes=[CC])
            tC = sb(est, "tC", [128, 512], F32)
            P.op("gpsimd", lambda e: e.affine_select(out=tC[:], in_=tmpf[:], pattern=[[0, 4], [1, 128]],
                                                     compare_op=ALU.is_gt, fill=0.0, base=0,
                                                     channel_multiplier=-1), reads=[CC, CD], writes=[CC])
            P.op("vector", lambda e: e.tensor_copy(out=maskD[:], in_=tC[:]), reads=[CC, CD], writes=[CC])
            P.barrier()
            P.flush()

        def run_il(gens):
            gens = list(gens)
            while gens:
                for g in list(gens):
                    try:
                        next(g)
                    except StopIteration:
                        gens.remove(g)

        H1S = [P.buf("h1s%d" % i) for i in range(2)]; H2S = [P.buf("h2s%d" % i) for i in range(4)]
        XE = [P.buf("xes%d" % i) for i in range(4)]; YS = [P.buf("ys%d" % i) for i in range(2)]
        OUTB = [P.buf("out%d" % i) for i in range(2)]

        with ExitStack() as es:
            g_in = bc_load(es, "g_in", ln_in_g); bb_in = bc_load(es, "bb_in", ln_in_b)
            g_1 = bc_load(es, "g_1", ln1_g); bb_1 = bc_load(es, "bb_1", ln1_b)
            binp = sb(es, "binp", [128, 20], F32)
            wdwp = sb(es, "wdwp", [128, 4, 31], F32)
            bdwp = sb(es, "bdwp", [128, 4], F32)
            lcg = sb(es, "lcg", [128, 4], F32)
            lcb = sb(es, "lcb", [128, 4], F32)
            for t_, a_ in [(binp, b_in_p), (wdwp, w_dw_p), (bdwp, b_dw_p), (lcg, lncg_p), (lcb, lncb_p)]:
                P.dma("sync", lambda e, t_=t_, a_=a_: e.dma_start(out=t_[:], in_=a_), CD, stream=True)
            brow_f = sb(es, "brow_f", [1, 1536], F32)
            brow = sb(es, "brow", [1, 1536], BF16)
            P.dma("sync", lambda e: e.dma_start(out=brow_f[:, 0:512], in_=b_v), CD, stream=True)
            P.dma("sync", lambda e: e.dma_start(out=brow_f[:, 512:1536], in_=b_out), CD, stream=True)
            P.op("vector", lambda e: e.tensor_copy(out=brow[:], in_=brow_f[:]), reads=[CC, CD], writes=[CC])

            WIN = P.buf("w_in"); WOUT = P.buf("w_out")
            w_in_bf = load_w(es, "w_in_bf", w_in, D, 2560, WIN)
            w_out_bf = load_w(es, "w_out_bf", w_out, D, D, WOUT)

            st_t = sb(es, "st_t", [128, 12], F32); mv_t = sb(es, "mv_t", [128, 2], F32); rs_t = sb(es, "rs_t", [128, 2], F32)
            LNT = P.buf("lnt")
            lnt = (st_t, mv_t, rs_t)

            pT = [ps(es, "pT%d" % i, [128, 1024], BF16) for i in range(2)]
            pTB = [P.buf() for _ in range(2)]
            pmm = [ps(es, "pmm%d" % i, [128, 512], F32) for i in range(2)]
            pmmB = [P.buf() for _ in range(2)]
            pz = ps(es, "pz", [128, 512], F32); pzB = P.buf()
            pz2 = pmm[0]; pz2B = pmmB[0]
            pst2 = pmm[1]; pst2B = pmmB[1]
            pc = ps(es, "pc", [128, 512], F32); pcB = P.buf()
            po = ps(es, "po", [128, 2, 2, 128], F32); poB = P.buf()
            pst = ps(es, "pst", [128, 512], F32); pstB = P.buf()
            mmi = [0]

            def next_pmm():
                i = mmi[0] % 2
                mmi[0] += 1
                return pmm[i], pmmB[i]

            tpi = [0]

            def next_pT():
                i = tpi[0] % 2
                tpi[0] += 1
                return pT[i], pTB[i]

            hbs = [sb(es, "hb%d" % i, [128, D], BF16) for i in range(4)]; hbsB = [P.buf() for _ in range(4)]
            hb = hbs[0]; hbB = hbsB[0]
            lnts = [lnt] + [(sb(es, "st_t%d" % i, [128, 12], F32), sb(es, "mv_t%d" % i, [128, 2], F32), sb(es, "rs_t%d" % i, [128, 2], F32))
                            for i in range(3)]
            LNTs = [LNT] + [P.buf() for _ in range(3)]

            with ExitStack() as esm:
                g_m = bc_load(esm, "g_m", ln_mem_g); bb_m = bc_load(esm, "bb_m", ln_mem_b)
                xt = [sb(esm, "xt%d" % i, [128, D], F32) for i in range(2)]
                xtB = [P.buf("xt%d" % i) for i in range(2)]
                WK = P.buf("wk"); WV = P.buf("wv")
                wk_bf = load_w(esm, "wk_bf", w_k, D, D, WK)
                wv_bf = load_w(esm, "wv_bf", w_v, D, D, WV)
                memT = sb(esm, "memT", [128, 8, NM], BF16); memTB = P.buf()
                KMT = P.buf("kmt"); VM = P.buf("vm")
                mn = sb(esm, "mn", [128, D], F32); mnB = P.buf()
                for mt in range(2):
                    P.dma("sync", lambda e, mt=mt: e.dma_start(out=xt[mt][:], in_=mem[mt * 128:(mt + 1) * 128, :]), xtB[mt])
                    layer_norm_tile(xt[mt][:], mn[:], g_m, bb_m, xtB[mt], mnB, lnt, LNT)
                    P.op("scalar", lambda e: e.copy(out=hb[:], in_=mn[:]), reads=[mnB], writes=[hbB])
                    p_, pB_ = next_pT()
                    transpose_to(hb, hbB, memT[:, :, mt * 128:(mt + 1) * 128], memTB, p_, pB_)
                for oc in range(8):
                    p_, pB_ = next_pmm()
                    P.group("tensor", [(lambda e, kc=kc, oc=oc, p_=p_: e.matmul(
                        p_[:, 0:NM], lhsT=wk_bf[:, kc, oc * 128:(oc + 1) * 128], rhs=memT[:, kc, :],
                        start=(kc == 0), stop=(kc == 7))) for kc in range(8)], reads=[WK, memTB], writes=[pB_])
                    P.op("vector", lambda e, oc=oc, p_=p_: e.tensor_copy(out=KmT[:, oc, :], in_=p_[:, 0:NM]),
                         reads=[pB_], writes=[KMT])
                for mt in range(2):
                    for hf in range(2):
                        p_, pB_ = next_pmm()
                        P.group("tensor", [(lambda e, kc=kc, mt=mt, hf=hf, p_=p_: e.matmul(
                            p_[:, :], lhsT=memT[:, kc, mt * 128:(mt + 1) * 128], rhs=wv_bf[:, kc, hf * 512:(hf + 1) * 512],
                            start=(kc == 0), stop=(kc == 7))) for kc in range(8)], reads=[WV, memTB], writes=[pB_])
                        P.op("vector", lambda e, mt=mt, hf=hf, p_=p_: e.tensor_copy(out=Vm[:, mt, hf * 512:(hf + 1) * 512], in_=p_[:, :]),
                             reads=[pB_], writes=[VM])
                P.barrier()
                P.flush()

            if upto < 1:
                return nc
            h_res = sb(es, "h_res", [128, 4, D], F32); hresB = [P.buf("hres%d" % i) for i in range(4)]
            hT = sb(es, "hT", [128, 8, 512], BF16); hTB = P.buf()
            uwin = sb(es, "uwin", [128, 4, PADL + 512], F32); uB = P.buf()
            sg = sb(es, "sg", [128, 512], F32); sgB = P.buf()
            acc = sb(es, "acc", [128, 2048], F32); accB = [P.buf() for _ in range(4)]
            m_sb = sb(es, "m_sb", [128, 512], F32); msB = P.buf()
            rstd_c = sb(es, "rstd_c", [128, 512], F32); rcB = P.buf()
            qTm = [sb(es, "qTm%d" % i, [128, 4, 512], BF16) for i in range(2)]; qTB = P.buf()
            kT = sb(es, "kT", [128, 4, 640], BF16); kTB = P.buf()
            kTn = sb(es, "kTn", [128, 4, 640], BF16); kTnB = P.buf()
            vwin = sb(es, "vwin", [128, 5, 512], BF16); vB = P.buf()
            e1 = sb(es, "e1", [128, 512], F32); e1B = P.buf()
            sq = sb(es, "sq", [128, 512], F32); sqB = P.buf()
            spm = [sb(es, "spm%d" % i, [128, 512], BF16) for i in range(2)]; spmB = [P.buf() for _ in range(2)]
            spp = sb(es, "spp", [128, 512], BF16); sppB = P.buf()
            e1b = sb(es, "e1b", [128, 512], F32); e1bB = P.buf()
            wtmp = sb(es, "wtmp", [128, 512], BF16); wtB = P.buf()
            wsm = [sb(es, "wsm%d" % i, [128, 512], BF16) for i in range(2)]; wmB = [P.buf() for _ in range(2)]
            wsp = [sb(es, "wsp%d" % i, [128, 512], BF16) for i in range(2)]; wpB = [P.buf() for _ in range(2)]
            catT = sb(es, "catT", [128, 8, 512], BF16); catB = P.buf()
            h1t = [sb(es, "h1t%d" % i, [128, D], F32) for i in range(2)]; h1tB = [P.buf("h1t%d" % i) for i in range(2)]

            P.op("vector", lambda e: e.memset(qTm[0][:], 0.0), writes=[qTB])
            P.op("vector", lambda e: e.memset(qTm[1][:], 0.0), writes=[qTB])
            P.op("vector", lambda e: e.memset(uwin[:], 0.0), writes=[uB])
            P.op("vector", lambda e: e.memset(kT[:], 0.0), writes=[kTB])
            P.op("vector", lambda e: e.memset(kTn[:], 0.0), writes=[kTnB])
            P.op("vector", lambda e: e.memset(vwin[:], 0.0), writes=[vB])

            def tile_in_gen(st, j):
                ti = st * 4 + j
                yield P.dma("sync", lambda e: e.dma_start(out=h_res[:, j, :], in_=x[ti * 128:(ti + 1) * 128, :]), hresB[j])
                yield from ln_gen(h_res[:, j, :], h_res[:, j, :], g_in, bb_in, hresB[j], hresB[j], lnts[j], LNTs[j])
                yield P.op("scalar", lambda e: e.copy(out=hbs[j][:], in_=h_res[:, j, :]), reads=[hresB[j]], writes=[hbsB[j]])
                p_, pB_ = next_pT()
                transpose_to(hbs[j], hbsB[j], hT[:, :, j * 128:(j + 1) * 128], hTB, p_, pB_)
                yield

            def conv_gen(st):
                for c in range(4):
                    cs = slice(c * 512, (c + 1) * 512)
                    yield P.op("vector", lambda e, c=c, cs=cs: e.tensor_scalar(
                        out=acc[:, cs], in0=uwin[:, c, 0:512], scalar1=wdwp[:, c, 0:1],
                        scalar2=bdwp[:, c:c + 1], op0=ALU.mult, op1=ALU.add), reads=[uB, CC, CD], writes=[accB[c]])
                    for jt in range(1, 31):
                        yield P.op("vector", lambda e, c=c, cs=cs, jt=jt: e.scalar_tensor_tensor(
                            out=acc[:, cs], in0=uwin[:, c, jt:jt + 512], scalar=wdwp[:, c, jt:jt + 1], in1=acc[:, cs],
                            op0=ALU.mult, op1=ALU.add), reads=[uB, CC, CD, accB[c]], writes=[accB[c]])
                yield P.group("tensor", [(lambda e, c=c: e.matmul(pst[:, :], lhsT=onesS[:], rhs=acc[:, c * 512:(c + 1) * 512],
                                                                  start=(c == 0), stop=(c == 3))) for c in range(4)],
                              reads=accB + [CC, CD], writes=[pstB])
                yield P.op("scalar", lambda e: e.copy(out=m_sb[:], in_=pst[:, :]), reads=[pstB], writes=[msB])
                for c in range(4):
                    cs = slice(c * 512, (c + 1) * 512)
                    yield P.op("gpsimd", lambda e, cs=cs: e.tensor_tensor(out=sq[:], in0=acc[:, cs], in1=acc[:, cs], op=ALU.mult),
                               reads=[accB[c]], writes=[sqB])
                    yield P.op("tensor", lambda e, c=c: e.matmul(pst2[:, :], lhsT=onesS[:], rhs=sq[:], start=(c == 0), stop=(c == 3)),
                               reads=[sqB, CC, CD], writes=[pst2B])
                yield P.op("gpsimd", lambda e: e.tensor_tensor(out=sq[:], in0=m_sb[:], in1=m_sb[:], op=ALU.mult), reads=[msB], writes=[sqB])
                yield P.op("vector", lambda e: e.tensor_tensor(out=rstd_c[:], in0=pst2[:, :], in1=sq[:], op=ALU.subtract),
                           reads=[pst2B, sqB], writes=[rcB])
                yield P.op("scalar", lambda e: e.activation(out=rstd_c[:], in_=rstd_c[:], func=AF.Ln, bias=EPS_T[:, 0:1]),
                           reads=[rcB, CC, CD], writes=[rcB])
                yield P.op("scalar", lambda e: e.activation(out=rstd_c[:], in_=rstd_c[:], func=AF.Exp, scale=-0.5), reads=[rcB], writes=[rcB])
                for c in range(4):
                    cs = slice(c * 512, (c + 1) * 512)
                    yield P.op("gpsimd", lambda e, cs=cs: e.tensor_tensor(out=acc[:, cs], in0=acc[:, cs], in1=m_sb[:], op=ALU.subtract),
                               reads=[accB[c], msB], writes=[accB[c]])
                    yield P.op("vector", lambda e, cs=cs: e.tensor_tensor(out=acc[:, cs], in0=acc[:, cs], in1=rstd_c[:], op=ALU.mult),
                               reads=[accB[c], rcB], writes=[accB[c]])
                    yield P.op("scalar", lambda e, c=c, cs=cs: e.activation(out=catT[:, c, :], in_=acc[:, cs], func=AF.Silu,
                                                                            scale=lcg[:, c:c + 1], bias=lcb[:, c:c + 1]),
                               reads=[accB[c], CC, CD], writes=[catB])
                yield P.op("vector", lambda e: e.tensor_copy(out=uwin[:, :, 0:PADL], in_=uwin[:, :, 512:512 + PADL]),
                           reads=[uB] + accB, writes=[uB])

            def sb_gen(st):
                ui = 0
                for jq in range(4):
                    qi = st * 4 + jq
                    blocks = [1 + jq] + ([jq] if qi > 0 else [])
                    nb = len(blocks)
                    for hg in range(2):
                        s2 = ui % 2
                        ui += 1
                        wsm_, wmB_ = wsm[s2], wmB[s2]
                        wsp_, wpB_ = wsp[s2], wpB[s2]
                        spd_, spdB_ = spm[s2], spmB[s2]
                        for bi, blk in enumerate(blocks):
                            diag = (bi == 0)
                            kc0 = blk * 128
                            pz_, pzB_ = (pz, pzB) if diag else (pz2, pz2B)
                            fz = []
                            for hh in range(4):
                                h = hg * 4 + hh
                                c = h // 2
                                fz.append(lambda e, hh=hh, c=c, h=h, kc0=kc0, jq=jq, pz_=pz_: e.matmul(
                                    pz_[:, hh * 128:(hh + 1) * 128], lhsT=kT[:, c, kc0:kc0 + 128],
                                    rhs=qTm[h % 2][:, c, jq * 128:(jq + 1) * 128], start=True, stop=True))
                            yield P.group("tensor", fz, reads=[kTB, qTB], writes=[pzB_])
                            e1_, e1B_ = (e1, e1B) if diag else (e1b, e1bB)
                            yield P.op("scalar", lambda e, e1_=e1_, pz_=pz_: e.activation(out=e1_[:], in_=pz_[:, :], func=AF.Exp, scale=0.125),
                                       reads=[pzB_], writes=[e1B_])
                            if diag:
                                yield P.op("scalar", lambda e: e.activation(out=e1[:], in_=e1[:], func=AF.Ln, bias=ONE_T[:, 0:1]),
                                           reads=[e1B, CC, CD], writes=[e1B])
                                yield P.op("vector", lambda e, spd_=spd_: e.tensor_tensor(out=spd_[:], in0=e1[:], in1=maskD[:], op=ALU.mult),
                                           reads=[e1B, CC, CD], writes=[spdB_])
                                cur, curB = spd_, spdB_
                            else:
                                yield P.op("scalar", lambda e: e.activation(out=spp[:], in_=e1b[:], func=AF.Ln, bias=ONE_T[:, 0:1]),
                                           reads=[e1bB, CC, CD], writes=[sppB])
                                cur, curB = spp, sppB
                            fc = []
                            for hh in range(4):
                                h = hg * 4 + hh
                                c = h // 2
                                sl = slice(hh * 128, (hh + 1) * 128)
                                fc.append(lambda e, sl=sl, cur=cur: e.matmul(pc[:, sl], lhsT=triInc[:], rhs=cur[:, sl],
                                                                             start=True, stop=False))
                                if not diag:
                                    fc.append(lambda e, sl=sl, spd_=spd_: e.matmul(pc[:, sl], lhsT=ones_bf[:], rhs=spd_[:, sl],
                                                                                   start=False, stop=False))
                                fc.append(lambda e, sl=sl, c=c, h=h, kc0=kc0, jq=jq: e.matmul(
                                    pc[:, sl], lhsT=kTn[:, c, kc0:kc0 + 128],
                                    rhs=qTm[h % 2][:, c, jq * 128:(jq + 1) * 128], start=False, stop=True))
                            yield P.group("tensor", fc, reads=[curB, spdB_, kTnB, qTB, CC, CD], writes=[pcB])
                            if diag:
                                yield P.op("scalar", lambda e: e.activation(out=wtmp[:], in_=pc[:, :], func=AF.Exp, scale=-1.0),
                                           reads=[pcB], writes=[wtB])
                                yield P.op("vector", lambda e, wsm_=wsm_: e.tensor_tensor(out=wsm_[:], in0=wtmp[:], in1=maskD[:], op=ALU.mult),
                                           reads=[wtB, CC, CD], writes=[wmB_])
                            else:
                                yield P.op("scalar", lambda e, wsp_=wsp_: e.activation(out=wsp_[:], in_=pc[:, :], func=AF.Exp, scale=-1.0),
                                           reads=[pcB], writes=[wpB_])
                        fo = []
                        for hh in range(4):
                            h = hg * 4 + hh
                            cl = hh // 2
                            fo.append(lambda e, hh=hh, h=h, cl=cl, jq=jq, nb=nb, wsm_=wsm_: e.matmul(
                                po[:, hh % 2, cl, :], lhsT=vwin[:, 1 + jq, (h // 2) * 128:(h // 2 + 1) * 128],
                                rhs=wsm_[:, hh * 128:(hh + 1) * 128], start=True, stop=(nb == 1)))
                            if nb == 2:
                                fo.append(lambda e, hh=hh, h=h, cl=cl, jq=jq, wsp_=wsp_: e.matmul(
                                    po[:, hh % 2, cl, :], lhsT=vwin[:, jq, (h // 2) * 128:(h // 2 + 1) * 128],
                                    rhs=wsp_[:, hh * 128:(hh + 1) * 128], start=False, stop=True))
                        yield P.group("tensor", fo, reads=[vB, wmB_] + ([wpB_] if nb == 2 else []), writes=[poB])
                        yield P.op("scalar", lambda e, hg=hg, jq=jq: e.copy(
                            out=catT[0:64, 4 + hg * 2:6 + hg * 2, jq * 128:(jq + 1) * 128], in_=po[0:64, 0, :, :]),
                            reads=[poB], writes=[catB])
                        yield P.op("vector", lambda e, hg=hg, jq=jq: e.tensor_copy(
                            out=catT[64:128, 4 + hg * 2:6 + hg * 2, jq * 128:(jq + 1) * 128], in_=po[64:128, 1, :, :]),
                            reads=[poB], writes=[catB])
                yield P.op("gpsimd", lambda e: e.tensor_copy(out=kT[:, :, 0:128], in_=kT[:, :, 512:640]), reads=[kTB], writes=[kTB])
                yield P.op("gpsimd", lambda e: e.tensor_copy(out=kTn[:, :, 0:128], in_=kTn[:, :, 512:640]), reads=[kTnB], writes=[kTnB])
                yield P.op("gpsimd", lambda e: e.tensor_copy(out=vwin[:, 0, :], in_=vwin[:, 4, :]), reads=[vB], writes=[vB])

            def outproj_gen(st, j):
                ti = st * 4 + j
                pj = j % 2
                for hf in range(2):
                    p_, pB_ = next_pmm()
                    fns = [(lambda e, kc=kc, p_=p_, hf=hf: e.matmul(
                        p_[:, :], lhsT=catT[:, kc, j * 128:(j + 1) * 128], rhs=w_out_bf[:, kc, hf * 512:(hf + 1) * 512],
                        start=(kc == 0), stop=False)) for kc in range(8)]
                    fns.append(lambda e, p_=p_, hf=hf: e.matmul(p_[:, :], lhsT=ones_bf[0:1, :],
                                                         rhs=brow[0:1, 512 + hf * 512:512 + (hf + 1) * 512],
                                                         start=False, stop=True))
                    yield P.group("tensor", fns, reads=[WOUT, catB, CC, CD], writes=[pB_])
                    yield P.op("vector", lambda e, hf=hf, p_=p_: e.scalar_tensor_tensor(
                        out=acc[:, pj * 1024 + hf * 512:pj * 1024 + (hf + 1) * 512], in0=h_res[:, j, hf * 512:(hf + 1) * 512], scalar=ALPHA,
                        in1=p_[:, :], op0=ALU.mult, op1=ALU.add), reads=[hresB[j], pB_], writes=[accB[pj * 2 + hf]])
                yield from ln_gen(acc[:, pj * 1024:(pj + 1) * 1024], h1t[pj][:], g_1, bb_1, accB[pj * 2:pj * 2 + 2], h1tB[pj], lnts[j], LNTs[j])
                yield P.dma("gpsimd", lambda e: e.dma_start(out=h1s[ti * 128:(ti + 1) * 128, :], in_=h1t[pj][:]),
                            H1S[pj], reads=[h1tB[pj]], stream=True)

            for st in range(nst_run):
                run_il([tile_in_gen(st, j) for j in range(4)])
                for c in range(4):
                    p_, pB_ = next_pmm()
                    oc = 4 + c
                    P.group("tensor", [(lambda e, kc=kc, oc=oc, p_=p_: e.matmul(
                        p_[:, :], lhsT=w_in_bf[:, kc, oc * 128:(oc + 1) * 128], rhs=hT[:, kc, :],
                        start=(kc == 0), stop=(kc == 7))) for kc in range(8)], reads=[WIN, hTB], writes=[pB_])
                    P.op("scalar", lambda e, oc=oc, p_=p_: e.activation(out=sg[:], in_=p_[:, :], func=AF.Sigmoid,
                                                                          bias=binp[:, oc:oc + 1]),
                         reads=[pB_, CC, CD], writes=[sgB])
                    p2, pB2 = next_pmm()
                    oc2 = c
                    P.group("tensor", [(lambda e, kc=kc, oc2=oc2, p2=p2: e.matmul(
                        p2[:, :], lhsT=w_in_bf[:, kc, oc2 * 128:(oc2 + 1) * 128], rhs=hT[:, kc, :],
                        start=(kc == 0), stop=(kc == 7))) for kc in range(8)], reads=[WIN, hTB], writes=[pB2])
                    P.op("vector", lambda e, c=c, p2=p2: e.scalar_tensor_tensor(
                        out=uwin[:, c, PADL:PADL + 512], in0=p2[:, :], scalar=binp[:, c:c + 1], in1=sg[:],
                        op0=ALU.add, op1=ALU.mult), reads=[pB2, sgB, CC, CD], writes=[uB])
                for c in range(4):
                    p_, pB_ = next_pmm()
                    oc = 8 + c
                    P.group("tensor", [(lambda e, kc=kc, oc=oc, p_=p_: e.matmul(
                        p_[:, :], lhsT=w_in_bf[:, kc, oc * 128:(oc + 1) * 128], rhs=hT[:, kc, :],
                        start=(kc == 0), stop=(kc == 7))) for kc in range(8)], reads=[WIN, hTB], writes=[pB_])
                    P.op("scalar", lambda e, oc=oc, c=c, p_=p_: e.activation(out=qTm[0][0:64, c, :], in_=p_[0:64, :], func=AF.Identity,
                                                                               bias=binp[0:64, oc:oc + 1]),
                         reads=[pB_, CC, CD], writes=[qTB])
                    P.op("scalar", lambda e, oc=oc, c=c, p_=p_: e.activation(out=qTm[1][64:128, c, :], in_=p_[64:128, :], func=AF.Identity,
                                                                               bias=binp[64:128, oc:oc + 1]),
                         reads=[pB_, CC, CD], writes=[qTB])
                for c in range(4):
                    p_, pB_ = next_pmm()
                    oc = 12 + c
                    P.group("tensor", [(lambda e, kc=kc, oc=oc, p_=p_: e.matmul(
                        p_[:, :], lhsT=w_in_bf[:, kc, oc * 128:(oc + 1) * 128], rhs=hT[:, kc, :],
                        start=(kc == 0), stop=(kc == 7))) for kc in range(8)], reads=[WIN, hTB], writes=[pB_])
                    P.op("vector", lambda e, oc=oc, c=c, p_=p_: e.tensor_scalar(
                        out=kT[:, c, 128:640], in0=p_[:, :], scalar1=binp[:, oc:oc + 1], scalar2=None, op0=ALU.add),
                         reads=[pB_, CC, CD], writes=[kTB])
                    P.op("vector", lambda e, oc=oc, c=c, p_=p_: e.tensor_scalar(
                        out=kTn[:, c, 128:640], in0=p_[:, :], scalar1=binp[:, oc:oc + 1], scalar2=-0.125,
                        op0=ALU.add, op1=ALU.mult), reads=[pB_, CC, CD], writes=[kTnB])
                for j in range(4):
                    p_, pB_ = next_pmm()
                    fns = [(lambda e, kc=kc, j=j, p_=p_: e.matmul(
                        p_[:, :], lhsT=hT[:, kc, j * 128:(j + 1) * 128], rhs=w_in_bf[:, kc, 2048:2560],
                        start=(kc == 0), stop=False)) for kc in range(8)]
                    fns.append(lambda e, p_=p_: e.matmul(p_[:, :], lhsT=ones_bf[0:1, :], rhs=brow[0:1, 0:512],
                                                          start=False, stop=True))
                    P.group("tensor", fns, reads=[WIN, hTB, CC, CD], writes=[pB_])
                    P.op("scalar", lambda e, j=j, p_=p_: e.copy(out=vwin[:, 1 + j, :], in_=p_[:, :]), reads=[pB_], writes=[vB])
                run_il([conv_gen(st), sb_gen(st)])
                if dbg:
                    P.dma("sync", lambda e, st=st: e.dma_start(out=catd[st], in_=catT[:]), DBGB, reads=[catB], stream=True)
                    P.dma("sync", lambda e, st=st: e.dma_start(out=hd[st], in_=h_res[:]), DBGB, reads=hresB, stream=True)
                run_il([outproj_gen(st, j) for j in range(2)])
                run_il([outproj_gen(st, j) for j in range(2, 4)])
            P.barrier()
            P.flush()

        with ExitStack() as es:
          if upto >= 2:
            g_2 = bc_load(es, "g_2", ln2_g); bb_2 = bc_load(es, "bb_2", ln2_b)
            WQ = P.buf("wq"); WO = P.buf("wo"); WR = P.buf("wr")
            wq_bf = load_w(es, "wq_bf", w_q, D, D, WQ)
            wo_bf = load_w(es, "wo_bf", w_o, D, D, WO)
            wr_f = sb(es, "wr_f", [128, 8, NE], F32)
            P.dma("sync", lambda e: e.dma_start(out=wr_f[:], in_=w_r.rearrange("(c p) n -> p c n", p=128)), CD, stream=True)
            brr_f = sb(es, "brr_f", [1, NE], F32); brr = sb(es, "brr", [1, NE], BF16)
            P.dma("sync", lambda e: e.dma_start(out=brr_f[:], in_=b_r), CD, stream=True)
            P.op("vector", lambda e: e.tensor_copy(out=brr[:], in_=brr_f[:]), reads=[CC, CD], writes=[CC])
            eoff = sb(es, "eoff", [128, NE], F32)
            eoff_i = sb(es, "eoff_i", [128, NE], I32)
            P.op("gpsimd", lambda e: e.iota(eoff_i[:], pattern=[[CAP, NE]], base=0, channel_multiplier=0), writes=[CC])
            P.op("vector", lambda e: e.tensor_copy(out=eoff[:], in_=eoff_i[:]), reads=[CC, CD], writes=[CC])

            st_t = sb(es, "st_t", [128, 12], F32); mv_t = sb(es, "mv_t", [128, 2], F32); rs_t = sb(es, "rs_t", [128, 2], F32)
            LNT = P.buf("lntB")
            lnt = (st_t, mv_t, rs_t)
            pT = [ps(es, "pT%d" % i, [128, 1024], BF16) for i in range(1)]
            pTB = [P.buf() for _ in range(1)]
            pmm = [ps(es, "pmm%d" % i, [128, 512], F32) for i in range(3)]
            pmmB = [P.buf() for _ in range(3)]
            pS = [ps(es, "pS%d" % i, [128, 512], F32) for i in range(2)]
            pSB = [P.buf() for _ in range(2)]
            pSum = ps(es, "pSum", [128, 512], F32); pSumB = P.buf()
            pO = ps(es, "pO", [128, 512], F32); pOB = P.buf()
            mmi = [0]; tpi = [0]; psi = [0]

            def next_pmm():
                i = mmi[0] % 3
                mmi[0] += 1
                return pmm[i], pmmB[i]

            def next_pT():
                return pT[0], pTB[0]

            h1r = sb(es, "h1r", [128, 4, D], F32); h1rB = [P.buf("h1r%d" % j) for j in range(4)]
            hb = sb(es, "hbB", [128, D], BF16); hbB = P.buf()
            h1T = sb(es, "h1T", [128, 8, 512], BF16); h1TB = P.buf()
            qmT = sb(es, "qmT", [128, 8, 512], BF16); qmTB = P.buf()
            Et = [sb(es, "Et%d" % i, [128, 512], BF16) for i in range(2)]; EtB = [P.buf() for _ in range(2)]
            rinv = sb(es, "rinv", [128, 512], F32); rinvB = P.buf()
            OT = sb(es, "OT", [128, 8, 512], BF16); OTB = P.buf()
            pre = [sb(es, "preB%d" % i, [128, D], F32) for i in range(4)]; preB = [P.buf() for _ in range(4)]
            lnts = [lnt] + [(sb(es, "st_tB%d" % i, [128, 12], F32), sb(es, "mv_tB%d" % i, [128, 2], F32), sb(es, "rs_tB%d" % i, [128, 2], F32))
                            for i in range(3)]
            LNTs = [LNT] + [P.buf() for _ in range(3)]
            h2t = [sb(es, "h2t%d" % i, [128, D], F32) for i in range(4)]; h2tB = [P.buf("h2t%d" % i) for i in range(4)]
            h2b = [sb(es, "h2b%d" % i, [128, D], BF16) for i in range(4)]; h2bB = [P.buf("h2b%d" % i) for i in range(4)]
            h2T = [sb(es, "h2T%d" % i, [128, 8, 128], F32) for i in range(4)]; h2TB = [P.buf() for _ in range(4)]

            class RT:
                pass
            rts = []
            for i in range(4):
                r_ = RT()
                r_.lg = sb(es, "lg%d" % i, [128, NE], F32); r_.lgB = P.buf()
                r_.m8 = sb(es, "m8%d" % i, [128, 8], F32); r_.m8B = P.buf()
                r_.nm = sb(es, "nm%d" % i, [128, 1], F32)
                r_.msk = sb(es, "msk%d" % i, [128, NE], F32); r_.mskB = P.buf()
                r_.mskb = sb(es, "mskb%d" % i, [128, NE], BF16); r_.mskbB = P.buf()
                r_.ex = sb(es, "ex%d" % i, [128, NE], F32); r_.exB = P.buf()
                r_.ssum = sb(es, "ssum%d" % i, [128, 2], F32)
                r_.G = sb(es, "G%d" % i, [128, NE], F32); r_.GB = P.buf()
                r_.posf = sb(es, "posf%d" % i, [128, NE], F32); r_.posB = P.buf()
                r_.oh = sb(es, "oh%d" % i, [128, NE], F32); r_.ohB = P.buf()
                r_.tmp32 = sb(es, "tmp32%d" % i, [128, NE], F32); r_.t32B = P.buf()
                r_.dstf = sb(es, "dstf%d" % i, [128, 4], F32); r_.dstfB = P.buf()
                rts.append(r_)
            cum = sb(es, "cum", [128, NE], F32); cumB = P.buf()
            cumb = sb(es, "cumb", [128, NE], BF16); cumbB = P.buf()
            P.op("vector", lambda e: e.memset(cum[:], 0.0), writes=[cumB])
            P.op("vector", lambda e: e.memset(cumb[:], 0.0), writes=[cumbB])

            def b_tail_gen(st, j):
                ti = st * 4 + j
                R = rts[j]
                tb_, tbB_ = ([pmm[0], pmm[1], pmm[2], pO][j], [pmmB[0], pmmB[1], pmmB[2], pOB][j])

                def next_pmm():
                    return tb_, tbB_
                for hf in range(2):
                    p_, pB_ = next_pmm()
                    yield P.group("tensor", [(lambda e, kc=kc, hf=hf, p_=p_: e.matmul(
                        p_[:, :], lhsT=OT[:, kc, j * 128:(j + 1) * 128], rhs=wo_bf[:, kc, hf * 512:(hf + 1) * 512],
                        start=(kc == 0), stop=(kc == 7))) for kc in range(8)], reads=[WO, OTB], writes=[pB_])
                    yield P.op("vector", lambda e, hf=hf, p_=p_: e.scalar_tensor_tensor(
                        out=pre[j][:, hf * 512:(hf + 1) * 512], in0=h1r[:, j, hf * 512:(hf + 1) * 512], scalar=ALPHA,
                        in1=p_[:, :], op0=ALU.mult, op1=ALU.add), reads=[h1rB[j], pB_], writes=[preB[j]])
                yield from ln_gen(pre[j][:], h2t[j][:], g_2, bb_2, preB[j], h2tB[j], lnts[j], LNTs[j])
                yield P.dma("gpsimd", lambda e: e.dma_start(out=h2s[ti * 128:(ti + 1) * 128, :], in_=h2t[j][:]),
                            H2S[j], reads=[h2tB[j]], stream=True)
                yield P.op("scalar", lambda e: e.copy(out=h2b[j][:], in_=h2t[j][:]), reads=[h2tB[j]], writes=[h2bB[j]])
                for g4 in range(2):
                    pq, pqB = next_pmm()
                    P.group("tensor", [(lambda e, c=c, g4=g4, pq=pq: e.transpose(
                        out=pq[:, c * 128:(c + 1) * 128], in_=h2t[j][:, (g4 * 4 + c) * 128:(g4 * 4 + c + 1) * 128],
                        identity=ident_f[:])) for c in range(4)], reads=[h2tB[j], CC, CD], writes=[pqB])
                    yield P.op("vector", lambda e, g4=g4, pq=pq: e.tensor_copy(
                        out=h2T[j][:, g4 * 4:(g4 + 1) * 4, :], in_=pq[:, :].rearrange("p (c t) -> p c t", c=4)),
                        reads=[pqB], writes=[h2TB[j]])
                pl, plB = next_pmm()
                fns = [(lambda e, kc=kc, pl=pl: e.matmul(pl[:, 0:NE], lhsT=h2T[j][:, kc, :], rhs=wr_f[:, kc, :],
                                                         start=(kc == 0), stop=False)) for kc in range(8)]
                fns.append(lambda e, pl=pl: e.matmul(pl[:, 0:NE], lhsT=ones_f[0:1, :], rhs=brr_f[0:1, :], start=False, stop=True))
                P.group("tensor", fns, reads=[h2TB[j], CC, CD], writes=[plB])
                yield P.op("vector", lambda e, pl=pl: e.tensor_copy(out=R.lg[:], in_=pl[:, 0:NE]), reads=[plB], writes=[R.lgB])
                yield P.op("vector", lambda e: e.max(out=R.m8[:], in_=R.lg[:]), reads=[R.lgB], writes=[R.m8B])
                yield P.op("vector", lambda e: e.tensor_scalar(out=R.msk[:], in0=R.lg[:], scalar1=R.m8[:, 3:4], scalar2=None, op0=ALU.is_ge),
                           reads=[R.lgB, R.m8B], writes=[R.mskB])
                yield P.op("vector", lambda e: e.tensor_scalar(out=R.nm[:], in0=R.m8[:, 0:1], scalar1=-1.0, scalar2=None, op0=ALU.mult),
                           reads=[R.m8B], writes=[R.exB])
                yield P.op("scalar", lambda e: e.activation(out=R.ex[:], in_=R.lg[:], func=AF.Exp, bias=R.nm[:, 0:1]),
                           reads=[R.lgB, R.exB], writes=[R.exB])
                yield P.op("vector", lambda e: e.tensor_tensor(out=R.ex[:], in0=R.ex[:], in1=R.msk[:], op=ALU.mult),
                           reads=[R.exB, R.mskB], writes=[R.exB])
                yield P.op("vector", lambda e: e.reduce_sum(out=R.ssum[:, 0:1], in_=R.ex[:], axis=AX.X), reads=[R.exB], writes=[R.GB])
                yield P.op("vector", lambda e: e.reciprocal(out=R.ssum[:, 1:2], in_=R.ssum[:, 0:1]), reads=[R.GB], writes=[R.GB])
                yield P.op("vector", lambda e: e.tensor_scalar(out=R.G[:], in0=R.ex[:], scalar1=R.ssum[:, 1:2], scalar2=None, op0=ALU.mult),
                           reads=[R.exB, R.GB], writes=[R.GB])
                yield P.op("vector", lambda e: e.tensor_copy(out=R.mskb[:], in_=R.msk[:]), reads=[R.mskB], writes=[R.mskbB])
                pp, ppB = next_pmm()
                P.group("tensor", [lambda e, pp=pp: e.matmul(pp[:, 0:NE], lhsT=maskD[:, 0:128], rhs=R.mskb[:], start=True, stop=False),
                                   lambda e, pp=pp: e.matmul(pp[:, 0:NE], lhsT=ones_bf[:], rhs=cumb[:], start=False, stop=True)],
                        reads=[R.mskbB, cumbB, CC, CD], writes=[ppB])
                P.op("vector", lambda e, pp=pp: e.tensor_tensor(out=R.posf[:], in0=pp[:, 0:NE], in1=eoff[:], op=ALU.add),
                     reads=[ppB, CC, CD], writes=[R.posB])
                P.op("vector", lambda e: e.tensor_tensor(out=cum[:], in0=cum[:], in1=R.msk[:], op=ALU.add), reads=[cumB, R.mskB], writes=[cumB])
                P.op("vector", lambda e: e.tensor_copy(out=cumb[:], in_=cum[:]), reads=[cumB], writes=[cumbB])
                yield
                for k in range(4):
                    col = ti * 4 + k
                    yield P.op("vector", lambda e, k=k: e.tensor_scalar(out=R.oh[:], in0=R.lg[:], scalar1=R.m8[:, k:k + 1], scalar2=None,
                                                                         op0=ALU.is_equal), reads=[R.lgB, R.m8B], writes=[R.ohB])
                    yield P.op("vector", lambda e: e.tensor_tensor(out=R.tmp32[:], in0=R.oh[:], in1=R.posf[:], op=ALU.mult),
                               reads=[R.ohB, R.posB], writes=[R.t32B])
                    yield P.op("vector", lambda e, k=k: e.reduce_sum(out=R.dstf[:, k:k + 1], in_=R.tmp32[:], axis=AX.X),
                               reads=[R.t32B], writes=[R.dstfB])
                    yield P.op("vector", lambda e: e.tensor_tensor(out=R.tmp32[:], in0=R.oh[:], in1=R.G[:], op=ALU.mult),
                               reads=[R.ohB, R.GB], writes=[R.t32B])
                    yield P.op("vector", lambda e, col=col: e.reduce_sum(out=gates_all[:, col:col + 1], in_=R.tmp32[:], axis=AX.X),
                               reads=[R.t32B], writes=[GD])
                yield P.op("vector", lambda e: e.tensor_copy(out=dest_all[:, ti * 4:ti * 4 + 4], in_=R.dstf[:]),
                           reads=[R.dstfB], writes=[GD])
                for k in range(4):
                    col = ti * 4 + k
                    yield P.dma("gpsimd", lambda e, col=col: e.indirect_dma_start(
                        out=Xe[:, :], out_offset=IndirectOffsetOnAxis(ap=dest_all[:, col:col + 1], axis=0),
                        in_=h2b[j][:], in_offset=None), XE[j], reads=[h2bB[j], GD], stream=True)

            for st in range(NST):
                for j in range(4):
                    ti = st * 4 + j
                    P.dma("sync", lambda e, ti=ti, j=j: e.dma_start(out=h1r[:, j, :], in_=h1s[ti * 128:(ti + 1) * 128, :]),
                          h1rB[j], reads=H1S)
                    P.op("scalar", lambda e, j=j: e.copy(out=hb[:], in_=h1r[:, j, :]), reads=[h1rB[j]], writes=[hbB])
                    p_, pB_ = next_pT()
                    transpose_to(hb, hbB, h1T[:, :, j * 128:(j + 1) * 128], h1TB, p_, pB_, eng="vector")
                for oc in range(8):
                    p_, pB_ = next_pmm()
                    P.group("tensor", [(lambda e, kc=kc, oc=oc, p_=p_: e.matmul(
                        p_[:, :], lhsT=wq_bf[:, kc, oc * 128:(oc + 1) * 128], rhs=h1T[:, kc, :],
                        start=(kc == 0), stop=(kc == 7))) for kc in range(8)], reads=[WQ, h1TB], writes=[pB_])
                    if oc % 2 == 0:
                        P.op("vector", lambda e, oc=oc, p_=p_: e.tensor_copy(out=qmT[:, oc, :], in_=p_[:, :]), reads=[pB_], writes=[qmTB])
                    else:
                        P.op("scalar", lambda e, oc=oc, p_=p_: e.copy(out=qmT[:, oc, :], in_=p_[:, :]), reads=[pB_], writes=[qmTB])
                for hd in range(4):
                    for mt in range(2):
                        P.group("tensor", [(lambda e, cc=cc, hd=hd, mt=mt: e.matmul(
                            pS[mt][:, :], lhsT=KmT[:, 2 * hd + cc, mt * 128:(mt + 1) * 128], rhs=qmT[:, 2 * hd + cc, :],
                            start=(cc == 0), stop=(cc == 1))) for cc in range(2)], reads=[KMT, qmTB], writes=[pSB[mt]])
                        P.op("scalar", lambda e, mt=mt: e.activation(out=Et[mt][:], in_=pS[mt][:, :], func=AF.Exp, scale=1.0 / 16.0),
                             reads=[pSB[mt]], writes=[EtB[mt]])
                    P.group("tensor", [(lambda e, mt=mt: e.matmul(pSum[:, :], lhsT=ones_bf[:], rhs=Et[mt][:],
                                                                   start=(mt == 0), stop=(mt == 1))) for mt in range(2)],
                            reads=[EtB[0], EtB[1], CC, CD], writes=[pSumB])
                    P.op("vector", lambda e: e.reciprocal(out=rinv[:], in_=pSum[:, :]), reads=[pSumB], writes=[rinvB])
                    for dc in range(2):
                        P.group("tensor", [(lambda e, mt=mt, hd=hd, dc=dc: e.matmul(
                            pO[:, :], lhsT=Vm[:, mt, hd * 256 + dc * 128:hd * 256 + (dc + 1) * 128], rhs=Et[mt][:],
                            start=(mt == 0), stop=(mt == 1))) for mt in range(2)], reads=[VM, EtB[0], EtB[1]], writes=[pOB])
                        P.op("vector", lambda e, hd=hd, dc=dc: e.tensor_tensor(out=OT[:, hd * 2 + dc, :], in0=pO[:, :], in1=rinv[:],
                                                                               op=ALU.mult), reads=[pOB, rinvB], writes=[OTB])
                run_il([b_tail_gen(st, j) for j in range(4)])
            pcn, pcnB = next_pmm()
            P.op("tensor", lambda e: e.matmul(pcn[:, 0:NE], lhsT=ones_bf[:], rhs=cumb[:], start=True, stop=True),
                 reads=[cumbB, CC, CD], writes=[pcnB])
            cnt_i = sb(es, "cnt_i", [128, NE], I32); cntB = P.buf("cnti")
            P.op("vector", lambda e: e.tensor_copy(out=cnt_i[:], in_=pcn[:, 0:NE]), reads=[pcnB], writes=[cntB])
            CNTD = P.buf("cntd")
            P.dma("sync", lambda e: e.dma_start(out=cntd[0:1, :], in_=cnt_i[0:1, :]), CNTD, reads=[cntB])
            P.cnt_ap = cntd
            P.barrier()
            P.flush()

        with ExitStack() as es:
          if upto >= 3:
            bgu = sb(es, "bgu", [128, NE, 16], F32)
            P.dma("sync", lambda e: e.dma_start(out=bgu[:], in_=b_gu_p), CD, stream=True)
            wgu_bf = [sb(es, "wgu%d" % i, [128, 8, 2048], BF16) for i in range(2)]; WGU = [P.buf("wgu%d" % i) for i in range(2)]
            wdn_bf = [sb(es, "wdn%d" % i, [128, 8, D], BF16) for i in range(2)]; WDN = [P.buf("wdn%d" % i) for i in range(2)]
            bdn_b = [sb(es, "bdnb%d" % i, [1, D], BF16) for i in range(2)]; BDNB = [P.buf("bdnb%d" % i) for i in range(2)]
            pT = [ps(es, "pT%d" % i, [128, 1024], BF16) for i in range(2)]
            pTB = [P.buf() for _ in range(2)]
            pg = [ps(es, "pg%d" % i, [128, 512], F32) for i in range(2)]; pgB = [P.buf() for _ in range(2)]
            pu = [ps(es, "pu%d" % i, [128, 512], F32) for i in range(2)]; puB = [P.buf() for _ in range(2)]
            py = [ps(es, "py%d" % i, [128, 512], F32) for i in range(2)]; pyB = [P.buf() for _ in range(2)]
            xe_t = [sb(es, "xe%d" % i, [128, D], BF16) for i in range(2)]; xeB = [P.buf("xe%d" % i) for i in range(2)]
            xT = [sb(es, "xT%d" % i, [128, 8, 512], BF16) for i in range(2)]; xTB = [P.buf() for _ in range(2)]
            actT = [sb(es, "actT%d" % i, [128, 8, 512], BF16) for i in range(2)]; actB = [P.buf() for _ in range(2)]
            gc = [sb(es, "gc%d" % i, [128, 512], F32) for i in range(2)]; gcB = [P.buf() for _ in range(2)]
            sgm = [sb(es, "sgm%d" % i, [128, 512], F32) for i in range(2)]; sgmB = [P.buf() for _ in range(2)]
            u1 = [sb(es, "u1%d" % i, [128, 512], F32) for i in range(2)]; u1B = [P.buf() for _ in range(2)]
            yt = [sb(es, "yt%d" % i, [128, D], F32) for i in range(2)]; ytB = [P.buf("yt%d" % i) for i in range(2)]
            tpi = [0]; xei = [0]; cnt2 = [0]; yi = [0]; pyi = [0]; bki = [0]

            def load_expert(e_):
                b = e_ % 2
                for c in range(8):
                    load_cast(w_gu[e_, c * 128:(c + 1) * 128, :], wgu_bf[b][:, c, :], WGU[b], 2048, eng="gpsimd")
                wv = w_dn[e_].rearrange("(c p) n -> p c n", p=128)
                for c in range(0, 8, 2):
                    load_cast(wv[:, c:c + 2, :], wdn_bf[b][:, c:c + 2, :], WDN[b], 2048, eng="gpsimd")
                P.dma("gpsimd", lambda e: e.dma_start(out=bdn_b[b][:], in_=b_dn[e_:e_ + 1, :]), BDNB[b], stream=True, war=True)

            def expert_block(e_, slot0, nsl):
                b = e_ % 2
                xb = bki[0] % 2
                bki[0] += 1
                for t in range(nsl // 128):
                    xi = xei[0] % 2
                    xei[0] += 1
                    r0 = e_ * CAP + slot0 + t * 128
                    P.dma("sync", lambda e, xi=xi, r0=r0: e.dma_start(out=xe_t[xi][:], in_=Xe[r0:r0 + 128, :]), xeB[xi], reads=XE)
                    i = tpi[0] % 2
                    tpi[0] += 1
                    transpose_to(xe_t[xi], xeB[xi], xT[xb][:, :, t * 128:(t + 1) * 128], xTB[xb], pT[i], pTB[i], eng="scalar")
                for jc in range(8):
                    k2 = cnt2[0] % 2
                    cnt2[0] += 1
                    P.group("tensor", [(lambda e, kc=kc, jc=jc, k2=k2: e.matmul(
                        pg[k2][:, 0:nsl], lhsT=wgu_bf[b][:, kc, jc * 128:(jc + 1) * 128], rhs=xT[xb][:, kc, 0:nsl],
                        start=(kc == 0), stop=(kc == 7))) for kc in range(8)], reads=[WGU[b], xTB[xb]], writes=[pgB[k2]])
                    P.group("tensor", [(lambda e, kc=kc, jc=jc, k2=k2: e.matmul(
                        pu[k2][:, 0:nsl], lhsT=wgu_bf[b][:, kc, 1024 + jc * 128:1024 + (jc + 1) * 128],
                        rhs=xT[xb][:, kc, 0:nsl], start=(kc == 0), stop=(kc == 7))) for kc in range(8)],
                        reads=[WGU[b], xTB[xb]], writes=[puB[k2]])
                    P.op("vector", lambda e, k2=k2, jc=jc: e.tensor_scalar(
                        out=gc[k2][:, 0:nsl], in0=pg[k2][:, 0:nsl], scalar1=bgu[:, e_, jc:jc + 1], scalar2=7.0,
                        op0=ALU.add, op1=ALU.min), reads=[pgB[k2], CC, CD], writes=[gcB[k2]])
                    P.op("scalar", lambda e, k2=k2: e.activation(out=sgm[k2][:, 0:nsl], in_=gc[k2][:, 0:nsl], func=AF.Sigmoid, scale=1.702),
                         reads=[gcB[k2]], writes=[sgmB[k2]])
                    P.op("vector", lambda e, k2=k2, jc=jc: e.tensor_scalar(
                        out=u1[k2][:, 0:nsl], in0=pu[k2][:, 0:nsl], scalar1=bgu[:, e_, 8 + jc:9 + jc], scalar2=-7.0,
                        op0=ALU.add, op1=ALU.max), reads=[puB[k2], CC, CD], writes=[u1B[k2]])
                    P.op("gpsimd", lambda e, k2=k2: e.tensor_tensor(out=gc[k2][:, 0:nsl], in0=gc[k2][:, 0:nsl], in1=sgm[k2][:, 0:nsl], op=ALU.mult),
                         reads=[gcB[k2], sgmB[k2]], writes=[gcB[k2]])
                    P.op("vector", lambda e, k2=k2: e.tensor_scalar(out=u1[k2][:, 0:nsl], in0=u1[k2][:, 0:nsl], scalar1=7.0, scalar2=1.0,
                                                                     op0=ALU.min, op1=ALU.add), reads=[u1B[k2]], writes=[u1B[k2]])
                    P.op("vector", lambda e, k2=k2, jc=jc: e.tensor_tensor(
                        out=actT[xb][:, jc, 0:nsl], in0=u1[k2][:, 0:nsl], in1=gc[k2][:, 0:nsl], op=ALU.mult),
                        reads=[u1B[k2], gcB[k2]], writes=[actB[xb]])
                for t in range(nsl // 128):
                    y2 = yi[0] % 2
                    yi[0] += 1
                    for hf in range(2):
                        p2 = pyi[0] % 2
                        pyi[0] += 1
                        fns = [(lambda e, jc=jc, t=t, hf=hf, p2=p2: e.matmul(
                            py[p2][:, :], lhsT=actT[xb][:, jc, t * 128:(t + 1) * 128], rhs=wdn_bf[b][:, jc, hf * 512:(hf + 1) * 512],
                            start=(jc == 0), stop=False)) for jc in range(8)]
                        fns.append(lambda e, hf=hf, p2=p2: e.matmul(py[p2][:, :], lhsT=ones_bf[0:1, :],
                                                                    rhs=bdn_b[b][0:1, hf * 512:(hf + 1) * 512], start=False, stop=True))
                        P.group("tensor", fns, reads=[actB[xb], WDN[b], BDNB[b], CC, CD], writes=[pyB[p2]])
                        P.op("scalar", lambda e, hf=hf, p2=p2, y2=y2: e.copy(out=yt[y2][:, hf * 512:(hf + 1) * 512], in_=py[p2][:, :]),
                             reads=[pyB[p2]], writes=[ytB[y2]])
                    r0 = e_ * CAP + slot0 + t * 128
                    P.dma("gpsimd", lambda e, r0=r0, y2=y2: e.dma_start(out=Ys[r0:r0 + 128, :], in_=yt[y2][:]), YS[y2],
                          reads=[ytB[y2]], stream=True)

            load_expert(0)
            P.regload(0, 0)
            P.regload(1, 1)
            for e_ in range(NE):
                if e_ + 1 < NE:
                    load_expert(e_ + 1)
                if e_ >= 1 and e_ + 1 < NE:
                    P.regload(e_ + 1, (e_ + 1) % 2)
                expert_block(e_, 0, 512)
                for slot0 in range(512, CAP, BLK):
                    P.begin_cond((e_, slot0, e_ % 2))
                    expert_block(e_, slot0, BLK)
                    P.end_cond()
            P.barrier()
            P.flush()

        with ExitStack() as es:
          if upto >= 4:
            g_3 = bc_load(es, "g_3", ln3_g); bb_3 = bc_load(es, "bb_3", ln3_b)
            st_t = sb(es, "st_t", [128, 12], F32); mv_t = sb(es, "mv_t", [128, 2], F32); rs_t = sb(es, "rs_t", [128, 2], F32)
            LNT = P.buf("lntF")
            lnt = (st_t, mv_t, rs_t)
            h2r = [sb(es, "h2r%d" % i, [128, D], F32) for i in range(2)]; h2rB = [P.buf("h2r%d" % i) for i in range(2)]
            yk = [[sb(es, "yk%d_%d" % (i, k), [128, D], F32) for k in range(4)] for i in range(2)]
            ykB = [[P.buf("yk%d_%d" % (i, k)) for k in range(4)] for i in range(2)]
            accF = [sb(es, "accF%d" % i, [128, D], F32) for i in range(2)]; accFB = [P.buf() for _ in range(2)]
            lnts = [lnt, (sb(es, "st_tF", [128, 12], F32), sb(es, "mv_tF", [128, 2], F32), sb(es, "rs_tF", [128, 2], F32))]
            LNTs = [LNT, P.buf()]
            ot = [sb(es, "ot%d" % i, [128, D], F32) for i in range(2)]; otB = [P.buf("ot%d" % i) for i in range(2)]

            def f_gen(ti):
                b = ti % 2
                yield P.dma("sync", lambda e: e.dma_start(out=h2r[b][:], in_=h2s[ti * 128:(ti + 1) * 128, :]), h2rB[b], reads=H2S)
                for k in range(4):
                    col = ti * 4 + k
                    yield P.dma("gpsimd", lambda e, col=col, k=k: e.indirect_dma_start(
                        out=yk[b][k][:], out_offset=None, in_=Ys[:, :],
                        in_offset=IndirectOffsetOnAxis(ap=dest_all[:, col:col + 1], axis=0)), ykB[b][k], reads=YS + [GD])
                yield P.op("scalar", lambda e: e.mul(out=accF[b][:], in_=h2r[b][:], mul=ALPHA), reads=[h2rB[b]], writes=[accFB[b]])
                for k in range(4):
                    col = ti * 4 + k
                    yield P.op("vector", lambda e, k=k, col=col: e.scalar_tensor_tensor(
                        out=accF[b][:], in0=yk[b][k][:], scalar=gates_all[:, col:col + 1], in1=accF[b][:], op0=ALU.mult, op1=ALU.add),
                        reads=[ykB[b][k], GD, accFB[b]], writes=[accFB[b]])
                yield from ln_gen(accF[b][:], ot[b][:], g_3, bb_3, accFB[b], otB[b], lnts[b], LNTs[b])
                yield P.dma("sync", lambda e: e.dma_start(out=out[ti * 128:(ti + 1) * 128, :], in_=ot[b][:]), OUTB[b],
                            reads=[otB[b]], stream=True)

            for ti in range(0, NT, 2):
                run_il([f_gen(ti), f_gen(ti + 1)])
            P.barrier()
            P.flush()
    return nc


_NC_CACHE = {}


def kernel(**inputs):
    f = lambda a: np.ascontiguousarray(np.asarray(a, dtype=np.float32))
    x = f(inputs["x"]); mem = f(inputs["mem"])
    B = x.shape[0]
    b_in = f(inputs["b_in"])[0]
    w_dw = f(inputs["w_dw"])[0]
    shared = {
        "ln_in_g": f(inputs["ln_in_g"]), "ln_in_b": f(inputs["ln_in_b"]),
        "ln_mem_g": f(inputs["ln_mem_g"]), "ln_mem_b": f(inputs["ln_mem_b"]),
        "w_in": f(inputs["w_in"])[0],
        "b_in_p": np.ascontiguousarray(b_in.reshape(20, 128).T),
        "b_v": np.ascontiguousarray(b_in[2048:2560].reshape(1, 512)),
        "w_dw_p": np.ascontiguousarray(w_dw.T.reshape(4, 128, 31).transpose(1, 0, 2)),
        "b_dw_p": np.ascontiguousarray(f(inputs["b_dw"])[0].reshape(4, 128).T),
        "lncg_p": np.ascontiguousarray(f(inputs["ln_conv_g"])[0].reshape(4, 128).T),
        "lncb_p": np.ascontiguousarray(f(inputs["ln_conv_b"])[0].reshape(4, 128).T),
        "w_out": f(inputs["w_out"])[0], "b_out": f(inputs["b_out"])[0].reshape(1, D),
        "ln1_g": f(inputs["ln1_g"])[0], "ln1_b": f(inputs["ln1_b"])[0],
        "w_q": f(inputs["w_q_mem"])[0], "w_k": f(inputs["w_k_mem"])[0],
        "w_v": f(inputs["w_v_mem"])[0], "w_o": f(inputs["w_o_mem"])[0],
        "ln2_g": f(inputs["ln2_g"])[0], "ln2_b": f(inputs["ln2_b"])[0],
        "w_r": f(inputs["w_router"])[0], "b_r": f(inputs["b_router"])[0].reshape(1, NE),
        "w_gu": f(inputs["w_gu"])[0],
        "b_gu_p": np.ascontiguousarray(f(inputs["b_gu"])[0].reshape(NE, 16, 128).transpose(2, 0, 1)),
        "w_dn": f(inputs["w_down"])[0], "b_dn": f(inputs["b_down"])[0],
        "ln3_g": f(inputs["ln3_g"])[0], "ln3_b": f(inputs["ln3_b"])[0],
    }
    if "nc" not in _NC_CACHE:
        _NC_CACHE["nc"] = build_program()
    nc = _NC_CACHE["nc"]
    in_maps = []
    for b in range(B):
        m = dict(shared)
        m["x"] = np.ascontiguousarray(x[b])
        m["mem"] = np.ascontiguousarray(mem[b])
        in_maps.append(m)
    res = run_bass_kernel_spmd(nc, in_maps, core_ids=list(range(B)))
    return np.stack([np.asarray(r["out"], dtype=np.float32) for r in res.results], axis=0)
```
